# Optimizing a Trainium2 kernel written in Bass

```python
import jax, jax.numpy as jnp
from jax import lax
import numpy as np

D_MODEL = 2048
BATCH = 4
SEQ = 2048
DEPTH = 2

POOL_WINDOWS = (2, 4, 8, 16)
N_POOL_GROUPS = len(POOL_WINDOWS)
D_POOL = D_MODEL // 2
POOL_GROUP = D_POOL // N_POOL_GROUPS
D_CONV = D_MODEL // 2
N_CONV_HEADS = 8
CONV_HEAD = D_CONV // N_CONV_HEADS
CONV_WIDTH = 3
D_MIX_EVEN = D_POOL + D_CONV
D_IN_EVEN = D_POOL + 3 * D_CONV
CHUNK = 128
D_SGU = D_MODEL
N_SGU_HEADS = 8
SGU_HEAD = D_SGU // N_SGU_HEADS
N_EXPERTS = 16
N_EXPERT_GROUPS = 4
EXPERTS_PER_GROUP = N_EXPERTS // N_EXPERT_GROUPS
TOP_K = 2
D_EXPERT = 512
N_MOD = 6
EPS = 1e-6
N_EVEN = (DEPTH + 1) // 2
N_ODD = DEPTH // 2

kernel_name = "hybrid_pool_conv_sgu_grouped_moe_adaln"


def rmsnorm(x, g):
    xf = x.astype(jnp.float32)
    y = xf * lax.rsqrt(jnp.mean(xf * xf, axis=-1, keepdims=True) + EPS)
    return (y * g.astype(jnp.float32)).astype(x.dtype)


def modulate(x, g, shift, scale):
    return rmsnorm(x, g) * (1 + scale[:, None, :]) + shift[:, None, :]


def pool_mixer(u, w_pool, pool_scale):
    b, s, _ = u.shape
    ug = u.astype(jnp.float32).reshape(b, s, N_POOL_GROUPS, POOL_GROUP)
    cs = jnp.cumsum(ug, axis=1)
    pos = jnp.arange(1, s + 1, dtype=jnp.float32)
    means = []
    for g, w in enumerate(POOL_WINDOWS):
        cg = cs[:, :, g]
        lag = jnp.pad(cg, ((0, 0), (w, 0), (0, 0)))[:, :s]
        means.append((cg - lag) / jnp.minimum(pos, float(w))[None, :, None])
    pooled = (jnp.stack(means, axis=2) - ug).astype(u.dtype)
    y = jnp.einsum('bsgc,gcd->bsgd', pooled, w_pool).reshape(b, s, D_POOL)
    return y * pool_scale


def conv_mixer(h, gate_b, gate_c, conv_w):
    b, s, _ = h.shape
    v = (gate_c * h).reshape(b, s, N_CONV_HEADS, CONV_HEAD)
    vp = jnp.pad(v, ((0, 0), (CONV_WIDTH - 1, 0), (0, 0), (0, 0)))
    z = sum(conv_w[k] * vp[:, k:k + s] for k in range(CONV_WIDTH))
    return gate_b * z.reshape(b, s, D_CONV)


def sgu_mixer(z, sgu_norm, w_spatial, b_spatial):
    b, s, _ = z.shape
    z = jax.nn.gelu(z)
    u, v = jnp.split(z, 2, axis=-1)
    v = rmsnorm(v, sgu_norm)
    n = s // CHUNK
    v = v.reshape(b, n, CHUNK, N_SGU_HEADS, SGU_HEAD)
    causal = jnp.tril(jnp.ones((CHUNK, CHUNK), dtype=w_spatial.dtype))
    w = w_spatial * causal[None]
    mixed = jnp.einsum('hts,bnshd->bnthd', w, v) + jnp.transpose(b_spatial)[None, None, :, :, None]
    return u * mixed.reshape(b, s, D_SGU)


def grouped_moe(h, w_router, router_bias, w_gate, w_up, w_down):
    b, s, d = h.shape
    t = h.reshape(b * s, d)
    aff = jax.nn.sigmoid(jnp.matmul(t, w_router).astype(jnp.float32))
    sel = aff + router_bias.astype(jnp.float32)
    grp = sel.reshape(-1, N_EXPERT_GROUPS, EXPERTS_PER_GROUP)
    group_score = jnp.sum(lax.top_k(grp, TOP_K)[0], axis=-1)
    best = jnp.argmax(group_score, axis=-1)
    in_group = (jnp.arange(N_EXPERTS) // EXPERTS_PER_GROUP)[None, :] == best[:, None]
    _, idx = lax.top_k(jnp.where(in_group, sel, -jnp.inf), TOP_K)
    wts = jnp.take_along_axis(aff, idx, axis=-1)
    wts = wts / jnp.sum(wts, axis=-1, keepdims=True)
    combine = jnp.sum(jax.nn.one_hot(idx, N_EXPERTS, dtype=jnp.float32) * wts[..., None], axis=1)
    g = jnp.einsum('td,edf->tef', t, w_gate)
    u = jnp.einsum('td,edf->tef', t, w_up)
    a = jax.nn.silu(g) * u * combine.astype(t.dtype)[..., None]
    y = jnp.einsum('tef,efd->td', a, w_down)
    return y.reshape(b, s, d)


def setup_inputs(seed: int = 0) -> dict:
    key = jax.random.key(seed)
    ks = jax.random.split(key, 24)
    f32 = jnp.float32
    nrm = lambda k, shape, scale: jax.random.normal(k, shape, f32) * scale
    D = D_MODEL
    return {
        "x": nrm(ks[0], (BATCH, SEQ, D), 1.0),
        "c": nrm(ks[1], (BATCH, D), 1.0),
        "ada_w": nrm(ks[2], (DEPTH, D, N_MOD * D), 0.5 * D ** -0.5),
        "ada_b": nrm(ks[3], (DEPTH, N_MOD * D), 0.02),
        "norm_mix": 1.0 + nrm(ks[4], (DEPTH, D), 0.02),
        "norm_ffn": 1.0 + nrm(ks[5], (DEPTH, D), 0.02),
        "w_in_even": nrm(ks[6], (N_EVEN, D, D_IN_EVEN), D ** -0.5),
        "w_pool": nrm(ks[7], (N_EVEN, N_POOL_GROUPS, POOL_GROUP, POOL_GROUP), POOL_GROUP ** -0.5),
        "pool_scale": 1.0 + nrm(ks[8], (N_EVEN, D_POOL), 0.02),
        "conv_w": nrm(ks[9], (N_EVEN, CONV_WIDTH, N_CONV_HEADS, CONV_HEAD), CONV_WIDTH ** -0.5),
        "w_out_even": nrm(ks[10], (N_EVEN, D_MIX_EVEN, D), D_MIX_EVEN ** -0.5),
        "w_in_odd": nrm(ks[11], (N_ODD, D, 2 * D_SGU), D ** -0.5),
        "sgu_norm": 1.0 + nrm(ks[12], (N_ODD, D_SGU), 0.02),
        "w_spatial": nrm(ks[13], (N_ODD, N_SGU_HEADS, CHUNK, CHUNK), CHUNK ** -0.5),
        "b_spatial": nrm(ks[14], (N_ODD, N_SGU_HEADS, CHUNK), 0.02),
        "w_out_odd": nrm(ks[15], (N_ODD, D_SGU, D), D_SGU ** -0.5),
        "w_router": nrm(ks[16], (D, N_EXPERTS), D ** -0.5),
        "router_bias": nrm(ks[17], (N_EXPERTS,), 0.01),
        "w_gate": nrm(ks[18], (DEPTH, N_EXPERTS, D, D_EXPERT), D ** -0.5),
        "w_up": nrm(ks[19], (DEPTH, N_EXPERTS, D, D_EXPERT), D ** -0.5),
        "w_down": nrm(ks[20], (DEPTH, N_EXPERTS, D_EXPERT, D), D_EXPERT ** -0.5),
        "final_norm": 1.0 + nrm(ks[21], (D,), 0.02),
    }


def reference(x, c, ada_w, ada_b, norm_mix, norm_ffn, w_in_even, w_pool, pool_scale, conv_w,
              w_out_even, w_in_odd, sgu_norm, w_spatial, b_spatial, w_out_odd, w_router,
              router_bias, w_gate, w_up, w_down, final_norm):
    c_act = jax.nn.silu(c)
    for l in range(DEPTH):
        mod = jnp.matmul(c_act, ada_w[l]) + ada_b[l]
        shift_m, scale_m, gate_m, shift_f, scale_f, gate_f = jnp.split(mod, N_MOD, axis=-1)
        h = modulate(x, norm_mix[l], shift_m, scale_m)
        if l % 2 == 0:
            i = l // 2
            z = jnp.matmul(h, w_in_even[i])
            z_pool, z_h, z_b, z_c = jnp.split(
                z, [D_POOL, D_POOL + D_CONV, D_POOL + 2 * D_CONV], axis=-1)
            y_a = pool_mixer(z_pool, w_pool[i], pool_scale[i])
            y_b = conv_mixer(z_h, z_b, z_c, conv_w[i])
            y = jnp.matmul(jnp.concatenate([y_a, y_b], axis=-1), w_out_even[i])
        else:
            i = l // 2
            z = jnp.matmul(h, w_in_odd[i])
            y = jnp.matmul(sgu_mixer(z, sgu_norm[i], w_spatial[i], b_spatial[i]), w_out_odd[i])
        x = x + gate_m[:, None, :] * y
        h = modulate(x, norm_ffn[l], shift_f, scale_f)
        x = x + gate_f[:, None, :] * grouped_moe(h, w_router, router_bias, w_gate[l], w_up[l], w_down[l])
    return rmsnorm(x, final_norm)
```

```python
import numpy as np
from contextlib import ExitStack
import concourse.bass as bass
import concourse.mybir as mybir
from concourse.bass_utils import run_bass_kernel_spmd

F32 = mybir.dt.float32
BF16 = mybir.dt.bfloat16
AF = mybir.ActivationFunctionType
ALU = mybir.AluOpType
AX = mybir.AxisListType

D = 2048
NT = 1024
KC = 16
HALO = 16
NE = 16
EPS = 1e-6
RING = 5
SLOT = 4096
NCORES = 8


class Eng:
    def __init__(self, name, nc, stack, nsem, limit=4000):
        self.name = name
        self.ops = []
        self.sems = [stack.enter_context(nc.semaphore(f"s_{name}_{i}")) for i in range(nsem)]
        self.cur = 0
        self.count = 0
        self.limit = limit
        self.waited = {}
        self.last = None

    def new_token(self):
        if self.count >= self.limit:
            self.cur += 1
            self.count = 0
        self.count += 1
        tok = (self.sems[self.cur], self.count, (self.name, self.cur), 1)
        self.last = tok
        return tok

    def wait(self, tok):
        if tok is None:
            return
        key = tok[2]
        if self.waited.get(key, 0) >= tok[1]:
            return
        self.waited[key] = tok[1]
        self.ops.append(("w", tok[0], tok[1]))


class DmaSem:
    def __init__(self, name, nc, stack):
        self.name = name
        self.sem = stack.enter_context(nc.semaphore(f"d_{name}"))
        self.count = 0

    def new_token(self):
        self.count += 1
        return (self.sem, 16 * self.count, ("dma", self.name), 16)


class Buf:
    def __init__(self, name, const=False):
        self.name = name
        self.w = None
        self.r = {}
        self.const = const


def _deps(E, reads, writes):
    for b in reads:
        if b.w is not None and not (E.name == "pe" and b.w[2][0] == "pe"):
            E.wait(b.w)
    for b in writes:
        if b.w is not None and not (E.name == "pe" and b.w[2][0] == "pe"):
            E.wait(b.w)
        for t in b.r.values():
            if not (E.name == "pe" and t[2][0] == "pe"):
                E.wait(t)


def _commit(tok, reads, writes, rkey):
    for b in reads:
        if not b.const:
            b.r[rkey] = tok
    for b in writes:
        b.w = tok
        b.r = {}


def emit(E, fn, reads=(), writes=(), dsem=None):
    _deps(E, reads, writes)
    if dsem is not None:
        tok = dsem.new_token()
        rkey = ("dma", dsem.name, dsem.count)
    else:
        tok = E.new_token()
        rkey = E.name
    E.ops.append(("i", fn, tok))
    _commit(tok, reads, writes, rkey)
    return tok


def emit_group(E, fns, reads=(), writes=()):
    _deps(E, reads, writes)
    tok = E.new_token()
    allreads = list(reads)
    for i, f in enumerate(fns):
        if isinstance(f, tuple):
            fn, rds = f
            _deps(E, rds, ())
            for r in rds:
                if r not in allreads:
                    allreads.append(r)
        else:
            fn = f
        E.ops.append(("i", fn, tok if i == len(fns) - 1 else None))
    _commit(tok, allreads, writes, E.name)
    return tok


def replay(E, h):
    for op in E.ops:
        if op[0] == "w":
            h.wait_ge(op[1], op[2])
        else:
            inst = op[1](h)
            if op[2] is not None:
                inst.then_inc(op[2][0], op[2][3])


def build(stop=None):
    nc = bass.Bass("TRN2", target_bir_lowering=False)

    def din(name, shape):
        return nc.dram_tensor(name, list(shape), F32, kind="ExternalInput").ap()

    xT_d = din("xT", [128, KC, NT])
    xhT_d = din("xhT", [128, KC, HALO])
    cT_d = din("cT", [128, KC])
    pc_d = din("pc", [128, 65])
    ada_w_d = din("ada_w", [2, D, 6 * D])
    ada_bT_d = din("ada_bT", [128, 2, 96])
    nmix_d = din("nmixT", [128, 2, KC])
    nffn_d = din("nffnT", [128, 2, KC])
    fnorm_d = din("fnormT", [128, KC])
    w_in_even_d = din("w_in_even", [D, 2 * D])
    w_pool_d = din("w_pool", [4, 256, 256])
    pscale_d = din("pscaleT", [128, 8])
    convw_d = din("convwT", [128, 3, 8])
    w_out_even_d = din("w_out_even", [D, D])
    w_in_odd_d = din("w_in_odd", [D, 2 * D])
    sgn_d = din("sgu_norm", [1, D])
    wsp_d = din("w_spatial", [8, 128, 128])
    bsp_d = din("b_spatial", [1, 8 * 128])
    w_out_odd_d = din("w_out_odd", [D, D])
    wr_d = din("w_router", [D, NE])
    rb_d = din("router_bias", [1, NE])
    wg_d = din("w_gate", [2, NE, D, 512])
    wu_d = din("w_up", [2, NE, D, 512])
    wd_d = din("w_down", [2, NE, 512, D])
    ident_d = din("ident", [128, 128])
    tril_d = din("trilT", [128, 128])
    outT_d = nc.dram_tensor("outT", [128, KC, NT], F32, kind="ExternalOutput").ap()

    with ExitStack() as st:
        def sb(name, shape, dt):
            return st.enter_context(nc.sbuf_tensor("sb_" + name, list(shape), dt))

        xT = sb("xT", [128, KC, NT], F32)
        hT = sb("hT", [128, KC, NT], BF16)
        xh = sb("xh", [128, KC, HALO], F32)
        hh = sb("hh", [128, KC, HALO], BF16)
        ring = sb("ring", [128, RING, SLOT], BF16)
        arb = sb("arb", [128, 18432], BF16)
        arf = sb("arf", [128, 3136], F32)
        wspn = arf[:, 0:1024].rearrange("p (h s) -> p h s", h=8)
        cT = sb("cT", [128, KC], F32)
        cact = sb("cact", [128, KC], BF16)
        pc = sb("pc", [128, 65], F32)
        ada_bT = sb("ada_bT", [128, 2, 96], F32)
        modsb = sb("modsb", [128, 2, 96], F32)
        nmix = sb("nmix", [128, 2, KC], F32)
        nffn = sb("nffn", [128, 2, KC], F32)
        fnorm = sb("fnorm", [128, KC], F32)
        a1 = sb("a1", [128, 2, KC], F32)
        a2 = sb("a2", [128, 2, KC], F32)
        pscale = sb("pscale", [128, 8], F32)
        convw = sb("convw", [128, 3, 8], F32)
        wpool = sb("wpool", [128, 4, 2, 256], BF16)
        wspT = sb("wspT", [128, 8, 128], BF16)
        bmat = sb("bmat", [128, 8, 128], BF16)
        wr = sb("wr", [128, KC, NE], F32)
        rbias = sb("rbias", [128, NE], F32)
        ident = sb("ident", [128, 128], F32)
        tril = sb("tril", [128, 128], F32)
        ones_bf = sb("ones_bf", [128, 128], BF16)
        selmat = sb("selmat", [32, 2, 128], BF16)
        combT = sb("combThl", [32, 1024], BF16)
        epst = sb("epst", [128, 1], F32)
        rt = sb("rt", [128, 1040], F32)
        small = sb("small", [128, 64], F32)

        psb = [st.enter_context(nc.psum_tensor(f"ps{i}", [128, 512], F32)) for i in range(8)]

        PE = Eng("pe", nc, st, 6)
        ACT = Eng("act", nc, st, 4)
        DVE = Eng("dve", nc, st, 6)
        POOL = Eng("pool", nc, st, 2)
        SP = Eng("sp", nc, st, 2)
        slot_sems = [DmaSem(f"slot{i}", nc, st) for i in range(RING + 2)]
        par_sem = DmaSem("par", nc, st)
        parg_sem = DmaSem("parg", nc, st)
        x_sem = DmaSem("x", nc, st)
        misc_sem = DmaSem("misc", nc, st)
        out_sem = DmaSem("out", nc, st)

        block = st.enter_context(nc.Block())

        B_x = [[Buf(f"x{c}_{b}") for b in range(2)] for c in range(KC)]
        B_h = [Buf(f"h{c}") for c in range(KC)]
        B_hh = Buf("hh")
        B_slot = [Buf(f"slot{i}") for i in range(RING + 2)]
        B_ps = [Buf(f"ps{i}") for i in range(8)]
        B_par = Buf("par", const=True)
        B_mod = [Buf(f"mod{l}") for l in range(2)]
        state = {"main": 0, "aux": 0, "pos": 0, "ring": list(range(RING)), "gate": {}}

        def main_bank():
            i = state["main"]
            state["main"] = (i + 1) % 6
            return i

        def aux_bank():
            i = 6 + state["aux"]
            state["aux"] = (state["aux"] + 1) % 2
            return i

        def barrier():
            lasts = [PE.last, ACT.last, DVE.last]
            for e in (ACT, DVE):
                for t in lasts:
                    if t is not None:
                        e.wait(t)

        def set_ring(n, gate_toks=None):
            state["ring"] = list(range(n))
            state["pos"] = 0
            state["gate"] = {i: gate_toks for i in range(RING, n)} if gate_toks else {}

        def next_slot(src_ap, view):
            rl = state["ring"]
            i = rl[state["pos"] % len(rl)]
            state["pos"] += 1
            nelem = 1
            for s_ in view[1:]:
                nelem *= s_
            if i < RING:
                dst = ring[:, i, 0:nelem]
            else:
                o = 8192 + (i - RING) * SLOT
                dst = arb[:, o:o + nelem]
                if i in state["gate"]:
                    for t_ in state["gate"].pop(i):
                        POOL.wait(t_)
            if len(view) == 3:
                dst = dst.rearrange("p (a b) -> p a b", a=view[1])
            emit(POOL, lambda e, dst=dst, src=src_ap: e.dma_start(out=dst, in_=src),
                 reads=[], writes=[B_slot[i]], dsem=slot_sems[i])
            return B_slot[i], dst

        def spload(dst, src, sem=par_sem):
            emit(SP, lambda e, dst=dst, src=src: e.dma_start(out=dst, in_=src), dsem=sem)

        for c in range(KC):
            emit(SP, lambda e, c=c: e.dma_start(out=xT[:, c, :], in_=xT_d[:, c, :]),
                 writes=[B_x[c][0], B_x[c][1]], dsem=x_sem)
        xtok = (x_sem.sem, 16 * x_sem.count, ("dma", "x"), 16)
        for c in range(KC):
            for b in range(2):
                B_x[c][b].w = xtok
        spload(xh[:], xhT_d)
        spload(cT[:], cT_d)
        spload(pc[:], pc_d)
        spload(ada_bT[:], ada_bT_d)
        spload(nmix[:], nmix_d)
        spload(nffn[:], nffn_d)
        spload(fnorm[:], fnorm_d)
        spload(pscale[:], pscale_d)
        spload(convw[:], convw_d)
        spload(wspn, wsp_d.rearrange("h t s -> t h s"))
        spload(wr[:], wr_d.rearrange("(kc p) e -> p kc e", p=128))
        spload(rbias[:], rb_d[0, :].partition_broadcast(128))
        spload(ident[:], ident_d)
        spload(tril[:], tril_d)
        B_par.w = (par_sem.sem, 16 * par_sem.count, ("dma", "par"), 16)

        B_cst = Buf("cst")
        emit(DVE, lambda e: e.memset(ones_bf[:], 1.0), writes=[B_cst])
        emit(DVE, lambda e: e.memset(epst[:], EPS), writes=[B_cst])
        emit(DVE, lambda e: e.memset(bmat[:], 0.0), writes=[B_cst])
        emit(POOL, lambda e: e.dma_start(out=wpool[:], in_=w_pool_d.rearrange("g (kc p) n -> p g kc n", p=128)),
             dsem=parg_sem)
        emit(POOL, lambda e: e.dma_start(out=bmat[0:1, :, :], in_=bsp_d.rearrange("o (h t) -> o h t", h=8)),
             reads=[B_cst], dsem=parg_sem)
        B_parg = Buf("parg", const=True)
        B_parg.w = (parg_sem.sem, 16 * parg_sem.count, ("dma", "parg"), 16)
        B_cst.const = True

        emit(ACT, lambda e: e.activation(out=cact[:], in_=cT[:], func=AF.Silu), reads=[B_par], writes=[B_cst])

        B_wsp = Buf("wsp")
        for half in range(2):
            bk = aux_bank()
            fns = []
            for q in range(4):
                hd = half * 4 + q
                fns.append(lambda e, hd=hd, q=q, bk=bk: e.transpose(psb[bk][:, q * 128:(q + 1) * 128], wspn[:, hd, :], ident[:]))
            emit_group(PE, fns, reads=[B_par], writes=[B_ps[bk]])
            emit(DVE, lambda e, half=half, bk=bk: e.tensor_tensor(
                out=wspT[:, half * 4:(half + 1) * 4, :],
                in0=psb[bk][:].rearrange("p (q t) -> p q t", q=4),
                in1=tril[:].unsqueeze(1).to_broadcast([128, 4, 128]), op=ALU.mult),
                reads=[B_ps[bk], B_par], writes=[B_wsp])

        def ada_slots(l, j0, nslots):
            for s in range(j0, j0 + nslots):
                src = ada_w_d[l][:, 256 * s:256 * (s + 1)].rearrange("(kc p) n -> p kc n", p=128)
                bs, sv = next_slot(src, [128, KC, 256])
                bk = aux_bank()
                fns = []
                for q in range(2):
                    for kc in range(KC):
                        fns.append(lambda e, q=q, kc=kc, sv=sv, bk=bk: e.matmul(
                            psb[bk][:, q:q + 1], sv[:, kc, q * 128:(q + 1) * 128], cact[:, kc:kc + 1],
                            start=(kc == 0), stop=(kc == KC - 1)))
                emit_group(PE, fns, reads=[bs, B_cst], writes=[B_ps[bk]])
                emit(DVE, lambda e, s=s, bk=bk, l=l: e.tensor_tensor(
                    out=modsb[:, l, 2 * s:2 * s + 2], in0=psb[bk][:, 0:2], in1=ada_bT[:, l, 2 * s:2 * s + 2], op=ALU.add),
                    reads=[B_ps[bk], B_par], writes=[B_mod[l]])

        def mod(l, k):
            return modsb[:, l, 16 * k:16 * (k + 1)]

        sqv = arb[:, 16384:18432].rearrange("p (b t) -> p b t", b=2)
        rstd = arf[:, 0:1024]
        tmpv = arf[:, 1024:2048].rearrange("p (b t) -> p b t", b=2)
        hfv = arf[:, 2048:3072].rearrange("p (b t) -> p b t", b=2)
        lgT = rt[0:16, 0:1024]
        B_combT = Buf("combT")

        def rms_stats(tag, rd=None):
            if rd is None:
                rd = rstd
            B_sq = [Buf(f"sq{tag}{i}") for i in range(2)]
            B_rstd = Buf(f"rstd{tag}")
            bk0, bk1 = aux_bank(), aux_bank()
            bks = [bk0, bk1]
            for c in range(KC):
                i = c % 2
                if c % 4 < 2:
                    emit(ACT, lambda e, c=c, i=i: e.activation(out=sqv[:, i, :], in_=xT[:, c, :], func=AF.Square),
                         reads=[B_x[c][0], B_x[c][1]], writes=[B_sq[i]])
                else:
                    emit(DVE, lambda e, c=c, i=i: e.tensor_tensor(out=sqv[:, i, :], in0=xT[:, c, :], in1=xT[:, c, :], op=ALU.mult),
                         reads=[B_x[c][0], B_x[c][1]], writes=[B_sq[i]])
                fns = []
                for b in range(2):
                    fns.append(lambda e, b=b, i=i, c=c: e.matmul(
                        psb[bks[b]][:, :], ones_bf[:], sqv[:, i, b * 512:(b + 1) * 512],
                        start=(c == 0), stop=(c == KC - 1)))
                emit_group(PE, fns, reads=[B_sq[i], B_cst], writes=[B_ps[bk0], B_ps[bk1]])
            for b in range(2):
                emit(ACT, lambda e, b=b: e.activation(out=rd[:, b * 512:(b + 1) * 512], in_=psb[bks[b]][:, :],
                                                     func=AF.Ln, scale=1.0 / D, bias=epst[:, 0:1]),
                     reads=[B_ps[bks[b]], B_cst], writes=[B_rstd])
                emit(ACT, lambda e, b=b: e.activation(out=rd[:, b * 512:(b + 1) * 512], in_=rd[:, b * 512:(b + 1) * 512],
                                                     func=AF.Exp, scale=-0.5),
                     reads=[B_rstd], writes=[B_rstd])
            return B_rstd

        def dump_f32(src3):
            for c in range(KC):
                emit(SP, lambda e, c=c: e.dma_start(out=outT_d[:, c, :], in_=src3[:, c, :]),
                     reads=[B_x[c][0], B_x[c][1]], dsem=out_sem)

        def finish():
            SP.ops.append(("w", out_sem.sem, 16 * out_sem.count))

            @block.tensor
            def _(h):
                replay(PE, h)

            @block.scalar
            def _(h):
                replay(ACT, h)

            @block.vector
            def _(h):
                replay(DVE, h)

            @block.gpsimd
            def _(h):
                replay(POOL, h)

            @block.sync
            def _(h):
                replay(SP, h)

        def dump_bf16(src3, bufs):
            barrier()
            for c in range(KC):
                emit(DVE, lambda e, c=c: e.tensor_copy(out=xT[:, c, :], in_=src3[:, c, :]),
                     reads=bufs, writes=[B_x[c][0], B_x[c][1]])
            dump_f32(xT)

        B_hs = Buf("hs")
        sqh = arb[:, 0:256].rearrange("p (c t) -> p c t", c=KC)
        rsh = small[:, 0:16]
        th = rt[:, 0:256].rearrange("p (c t) -> p c t", c=KC)
        B_rstd0 = rms_stats("n1_0")
        emit(ACT, lambda e: e.activation(out=sqh, in_=xh[:], func=AF.Square), reads=[B_par], writes=[B_hs])
        bk = aux_bank()
        fns = [lambda e, c=c, bk=bk: e.matmul(psb[bk][:, 0:HALO], ones_bf[:], sqh[:, c, :],
                                              start=(c == 0), stop=(c == KC - 1)) for c in range(KC)]
        emit_group(PE, fns, reads=[B_hs, B_cst], writes=[B_ps[bk]])
        emit(ACT, lambda e, bk=bk: e.activation(out=rsh, in_=psb[bk][:, 0:HALO], func=AF.Sqrt,
                                                scale=1.0 / D, bias=epst[:, 0:1]),
             reads=[B_ps[bk], B_cst], writes=[B_hs])
        emit(DVE, lambda e: e.reciprocal(out=rsh, in_=rsh), reads=[B_hs], writes=[B_hs])
        emit(DVE, lambda e: e.tensor_tensor(out=th, in0=xh[:], in1=rsh.unsqueeze(1).to_broadcast([128, KC, HALO]),
                                            op=ALU.mult), reads=[B_hs, B_par], writes=[B_hs])
        ada_slots(0, 0, 16)
        for l in range(2):
            emit(DVE, lambda e, l=l: e.scalar_tensor_tensor(out=a1[:, l, :], in0=mod(l, 1), scalar=1.0, in1=nmix[:, l, :],
                                                            op0=ALU.add, op1=ALU.mult),
                 reads=[B_mod[l], B_par], writes=[B_mod[l]])
            if l == 0:
                B_rstd = B_rstd0
                emit(DVE, lambda e: e.tensor_tensor(out=th, in0=th, in1=a1[:, 0, :].unsqueeze(2).to_broadcast([128, KC, HALO]),
                                                    op=ALU.mult), reads=[B_hs, B_mod[0]], writes=[B_hs])
                emit(DVE, lambda e: e.tensor_tensor(out=hh[:], in0=th, in1=mod(0, 0).unsqueeze(2).to_broadcast([128, KC, HALO]),
                                                    op=ALU.add), reads=[B_hs, B_mod[0]], writes=[B_hh])
            else:
                barrier()
                B_rstd = rms_stats(f"n1_{l}")
            B_tmp = [Buf(f"tmp{i}") for i in range(2)]
            for c in range(KC):
                for b in range(2):
                    i = b
                    emit(DVE, lambda e, c=c, b=b, i=i, l=l: e.scalar_tensor_tensor(
                        out=tmpv[:, i, :], in0=xT[:, c, b * 512:(b + 1) * 512], scalar=a1[:, l, c:c + 1],
                        in1=rstd[:, b * 512:(b + 1) * 512], op0=ALU.mult, op1=ALU.mult),
                        reads=[B_x[c][b], B_rstd, B_mod[l]], writes=[B_tmp[i]])
                    emit(ACT, lambda e, c=c, b=b, i=i, l=l: e.activation(
                        out=hT[:, c, b * 512:(b + 1) * 512], in_=tmpv[:, i, :], func=AF.Identity,
                        bias=mod(l, 0)[:, c:c + 1], scale=1.0),
                        reads=[B_tmp[i], B_mod[l]], writes=[B_h[c]])
            if stop == f"h1_{l}":
                dump_bf16(hT, B_h)
                return nc, finish()
            barrier()

            ymix = arb[:, 0:16384].rearrange("p (c t) -> p c t", c=KC)
            B_ym = [Buf(f"ym{c}") for c in range(KC)]
            if l == 0:
                zb = arf[:, 0:1040]
                sA = arf[:, 1040:2080]
                sB = arf[:, 2080:3120]
                t16 = arf[:, 3120:3136]
                B_zb, B_sA, B_sB, B_t16 = Buf("zb"), Buf("sA"), Buf("sB"), Buf("t16")
                pooled = arb[:, 16384:18432].rearrange("p (c t) -> p c t", c=2)
                B_pl = [Buf("pl0"), Buf("pl1")]
                flag = pc[:, 0:1]

                def zgroup(sv, q, with_halo):
                    bks = [main_bank(), main_bank()]
                    fns = []
                    for kc in range(KC):
                        for b in range(2):
                            fns.append((lambda e, kc=kc, b=b, bks=bks: e.matmul(
                                psb[bks[b]][:, :], sv[:, kc, q * 128:(q + 1) * 128], hT[:, kc, b * 512:(b + 1) * 512],
                                start=(kc == 0), stop=(kc == KC - 1)), [B_h[kc]]))
                    bh = None
                    if with_halo:
                        bh = aux_bank()
                        for kc in range(KC):
                            fns.append(lambda e, kc=kc, bh=bh: e.matmul(
                                psb[bh][:, 0:HALO], sv[:, kc, q * 128:(q + 1) * 128], hh[:, kc, :],
                                start=(kc == 0), stop=(kc == KC - 1)))
                    return bks, bh, fns

                mod_sched = list(range(16, 32))
                def pool_mm(g):
                    for oc in range(2):
                        bks = [main_bank(), main_bank()]
                        fns = []
                        for kc in range(2):
                            for b in range(2):
                                fns.append(lambda e, kc=kc, b=b, oc=oc, g=g, bks=bks: e.matmul(
                                    psb[bks[b]][:, :], wpool[:, g, kc, oc * 128:(oc + 1) * 128],
                                    pooled[:, kc, b * 512:(b + 1) * 512], start=(kc == 0), stop=(kc == 1)))
                        emit_group(PE, fns, reads=[B_parg] + B_pl, writes=[B_ps[bks[0]], B_ps[bks[1]]])
                        for b in range(2):
                            emit(ACT, lambda e, b=b, oc=oc, g=g, bks=bks: e.activation(
                                out=ymix[:, 2 * g + oc, b * 512:(b + 1) * 512], in_=psb[bks[b]][:, :], func=AF.Identity,
                                scale=pscale[:, 2 * g + oc:2 * g + oc + 1]),
                                reads=[B_ps[bks[b]], B_par], writes=[B_ym[2 * g + oc]])
                for g in range(4):
                    w = 2 ** (g + 1)
                    for half in range(2):
                        c = 2 * g + half
                        if half == 0:
                            bs, sv = next_slot(w_in_even_d[:, 256 * g:256 * (g + 1)].rearrange("(kc p) n -> p kc n", p=128),
                                               [128, KC, 256])
                        bks, bh, fns = zgroup(sv, half, True)
                        emit_group(PE, fns, reads=[bs, B_hh], writes=[B_ps[bks[0]], B_ps[bks[1]], B_ps[bh]])
                        if half == 0 and g >= 1:
                            pool_mm(g - 1)
                        emit(ACT, lambda e, bh=bh: e.activation(out=zb[:, 0:HALO], in_=psb[bh][:, 0:HALO], func=AF.Identity,
                                                                scale=flag),
                             reads=[B_ps[bh], B_par], writes=[B_zb])
                        for b in range(2):
                            emit(ACT, lambda e, b=b, bks=bks: e.activation(
                                out=zb[:, HALO + b * 512:HALO + (b + 1) * 512], in_=psb[bks[b]][:, :], func=AF.Copy),
                                reads=[B_ps[bks[b]]], writes=[B_zb])
                        emit(DVE, lambda e: e.tensor_tensor(out=sA[:, 1:1040], in0=zb[:, 1:1040], in1=zb[:, 0:1039], op=ALU.add),
                             reads=[B_zb], writes=[B_sA])
                        cur, curB = sA, B_sA
                        oth, othB = sB, B_sB
                        k = 2
                        while k < w:
                            emit(DVE, lambda e, k=k, cur=cur, oth=oth: e.tensor_tensor(
                                out=oth[:, 2 * k - 1:1040], in0=cur[:, 2 * k - 1:1040], in1=cur[:, k - 1:1040 - k], op=ALU.add),
                                reads=[curB], writes=[othB])
                            cur, curB, oth, othB = oth, othB, cur, curB
                            k *= 2
                        emit(DVE, lambda e, cur=cur, half=half, w=w: e.scalar_tensor_tensor(
                            out=pooled[:, half, :], in0=cur[:, HALO:1040], scalar=1.0 / w, in1=zb[:, HALO:1040],
                            op0=ALU.mult, op1=ALU.subtract),
                            reads=[curB, B_zb], writes=[B_pl[half]])
                        emit(DVE, lambda e, cur=cur, g=g: e.tensor_tensor(
                            out=t16, in0=cur[:, HALO:2 * HALO], in1=pc[:, 1 + 16 * g:17 + 16 * g], op=ALU.mult),
                            reads=[curB, B_par], writes=[B_t16])
                        emit(DVE, lambda e, half=half: e.tensor_tensor(
                            out=pooled[:, half, 0:HALO], in0=t16, in1=zb[:, HALO:2 * HALO], op=ALU.subtract),
                            reads=[B_t16, B_zb], writes=[B_pl[half]])
                        ada_slots(0, mod_sched.pop(0), 1)
                zv = [arf[:, 0:1040], arf[:, 1040:2080]]
                accs = [arf[:, 2080:3104], rt[:, 0:1024]]
                B_zv = [B_zb, B_sA]
                B_ac = [B_sB, Buf("ac1")]

                def conv_slot(base, pr):
                    return next_slot(w_in_even_d[:, base + 256 * pr:base + 256 * (pr + 1)].rearrange("(kc p) n -> p kc n", p=128),
                                     [128, KC, 256])

                def some_ada(n):
                    for _ in range(n):
                        if mod_sched:
                            ada_slots(0, mod_sched.pop(0), 1)

                for pr in range(4):
                    sl = conv_slot(1024, pr)
                    for q in range(2):
                        bks, bh, fns = zgroup(sl[1], q, True)
                        emit_group(PE, fns, reads=[sl[0], B_hh], writes=[B_ps[bks[0]], B_ps[bks[1]], B_ps[bh]])
                        if pr == 0 and q == 0:
                            pool_mm(3)
                        emit(ACT, lambda e, bh=bh, q=q: e.activation(out=zv[q][:, 0:HALO], in_=psb[bh][:, 0:HALO], func=AF.Identity,
                                                                     scale=flag),
                             reads=[B_ps[bh], B_par], writes=[B_zv[q]])
                        for b in range(2):
                            emit(ACT, lambda e, b=b, bks=bks, q=q: e.activation(
                                out=zv[q][:, HALO + b * 512:HALO + (b + 1) * 512], in_=psb[bks[b]][:, :], func=AF.Copy),
                                reads=[B_ps[bks[b]]], writes=[B_zv[q]])
                    some_ada(0)
                    sl = conv_slot(3072, pr)
                    for q in range(2):
                        hd = 2 * pr + q
                        bks, bh, fns = zgroup(sl[1], q, True)
                        emit_group(PE, fns, reads=[sl[0], B_hh], writes=[B_ps[bks[0]], B_ps[bks[1]], B_ps[bh]])
                        emit(DVE, lambda e, bh=bh, q=q: e.tensor_tensor(out=zv[q][:, 0:HALO], in0=psb[bh][:, 0:HALO],
                                                                        in1=zv[q][:, 0:HALO], op=ALU.mult),
                             reads=[B_ps[bh], B_zv[q]], writes=[B_zv[q]])
                        for b in range(2):
                            emit(DVE, lambda e, b=b, bks=bks, q=q: e.tensor_tensor(
                                out=zv[q][:, HALO + b * 512:HALO + (b + 1) * 512], in0=psb[bks[b]][:, :],
                                in1=zv[q][:, HALO + b * 512:HALO + (b + 1) * 512], op=ALU.mult),
                                reads=[B_ps[bks[b]], B_zv[q]], writes=[B_zv[q]])
                        emit(ACT, lambda e, hd=hd, q=q: e.activation(out=accs[q], in_=zv[q][:, HALO:HALO + NT], func=AF.Identity,
                                                                     scale=convw[:, 2, hd:hd + 1]),
                             reads=[B_zv[q], B_par], writes=[B_ac[q]])
                        for k in (1, 0):
                            sh = 2 - k
                            emit(DVE, lambda e, hd=hd, k=k, sh=sh, q=q: e.scalar_tensor_tensor(
                                out=accs[q], in0=zv[q][:, HALO - sh:HALO - sh + NT], scalar=convw[:, k, hd:hd + 1],
                                in1=accs[q], op0=ALU.mult, op1=ALU.add),
                                reads=[B_zv[q], B_ac[q], B_par], writes=[B_ac[q]])
                    some_ada(0)
                    sl = conv_slot(2048, pr)
                    for q in range(2):
                        hd = 2 * pr + q
                        bks, bh, fns = zgroup(sl[1], q, False)
                        emit_group(PE, fns, reads=[sl[0]], writes=[B_ps[bks[0]], B_ps[bks[1]]])
                        for b in range(2):
                            emit(DVE, lambda e, b=b, bks=bks, hd=hd, q=q: e.tensor_tensor(
                                out=ymix[:, 8 + hd, b * 512:(b + 1) * 512], in0=psb[bks[b]][:, :],
                                in1=accs[q][:, b * 512:(b + 1) * 512], op=ALU.mult),
                                reads=[B_ps[bks[b]], B_ac[q]], writes=[B_ym[8 + hd]])
                    some_ada(2)
                while mod_sched:
                    ada_slots(0, mod_sched.pop(0), 1)
                w_out_d = w_out_even_d
            else:
                vq = arb[:, 0:16384].rearrange("p (c i f) -> p c i f", c=KC, i=8)
                ug = arb[:, 16384:17408].rearrange("p (b t) -> p b t", b=2)
                B_ug = [Buf("ug0"), Buf("ug1")]
                sgn = arf[:, 0:2048]
                B_sgn = Buf("sgn")
                for t_ in (PE.last, ACT.last, DVE.last):
                    SP.wait(t_)
                emit(SP, lambda e: e.dma_start(out=sgn, in_=sgn_d[0, :].partition_broadcast(128)), writes=[B_sgn], dsem=misc_sem)
                B_vq = [Buf(f"vq{c}") for c in range(KC)]
                sqj = arb[:, 17408:17920].rearrange("p (u q f) -> p u q f", u=2, q=2)
                B_sqj = [Buf("sqj0"), Buf("sqj1")]
                sspart = rt[:, 0:64].rearrange("p (i j) -> p i j", i=8)
                B_ssp = Buf("ssp")
                for j in range(8):
                    bs, sv = next_slot(w_in_odd_d[:, D + 256 * j:D + 256 * (j + 1)].rearrange("(kc p) n -> p kc n", p=128),
                                       [128, KC, 256])
                    for i in range(8):
                        bk = main_bank()
                        fns = [(lambda e, kc=kc, i=i, bk=bk, sv=sv: e.matmul(
                            psb[bk][:, 0:256], hT[:, kc, i * 128:(i + 1) * 128], sv[:, kc, :],
                            start=(kc == 0), stop=(kc == KC - 1)), [B_h[kc]]) for kc in range(KC)]
                        emit_group(PE, fns, reads=[bs], writes=[B_ps[bk]])
                        emit(ACT, lambda e, j=j, i=i, bk=bk: e.activation(
                            out=vq[:, 2 * j:2 * j + 2, i, :], in_=psb[bk][:, 0:256].rearrange("p (q f) -> p q f", q=2),
                            func=AF.Gelu_apprx_tanh),
                            reads=[B_ps[bk]], writes=[B_vq[2 * j], B_vq[2 * j + 1]])
                        emit(DVE, lambda e, j=j, i=i: e.scalar_tensor_tensor(
                            out=sqj[:, (i % 2), :, :], in0=vq[:, 2 * j:2 * j + 2, i, :], scalar=1.0, in1=vq[:, 2 * j:2 * j + 2, i, :],
                            op0=ALU.mult, op1=ALU.mult, accum_out=sspart[:, i, j:j + 1]),
                            reads=[B_vq[2 * j], B_vq[2 * j + 1]], writes=[B_sqj[i % 2], B_ssp])
                if stop == "vq0":
                    dump_bf16(arb[:, 0:16384].rearrange("p (c t) -> p c t", c=KC), B_vq)
                    return nc, finish()
                ssv = small[:, 16:24]
                rsv = small[:, 24:32]
                B_ssv = Buf("ssv")
                emit(DVE, lambda e: e.tensor_reduce(out=ssv, in_=sspart, axis=AX.X, op=ALU.add), reads=[B_ssp], writes=[B_ssv])
                emit(ACT, lambda e: e.activation(out=rsv, in_=ssv, func=AF.Sqrt, scale=1.0 / D, bias=epst[:, 0:1]),
                     reads=[B_ssv, B_cst], writes=[B_ssv])
                emit(DVE, lambda e: e.reciprocal(out=rsv, in_=rsv), reads=[B_ssv], writes=[B_ssv])

                def normalize_chunk(c):
                    emit(DVE, lambda e, c=c: e.tensor_tensor(out=vq[:, c, :, :], in0=vq[:, c, :, :],
                                                             in1=rsv.unsqueeze(2).to_broadcast([128, 8, 128]), op=ALU.mult),
                         reads=[B_vq[c], B_ssv], writes=[B_vq[c]])
                    emit(DVE, lambda e, c=c: e.tensor_tensor(out=vq[:, c, :, :], in0=vq[:, c, :, :],
                                                             in1=sgn[:, c * 128:(c + 1) * 128].unsqueeze(1).to_broadcast([128, 8, 128]),
                                                             op=ALU.mult),
                         reads=[B_vq[c], B_sgn], writes=[B_vq[c]])

                if stop == "vq1":
                    for c in range(KC):
                        normalize_chunk(c)
                    dump_bf16(arb[:, 0:16384].rearrange("p (c t) -> p c t", c=KC), B_vq)
                    return nc, finish()
                for j in range(8):
                    bs, sv = next_slot(w_in_odd_d[:, 256 * j:256 * (j + 1)].rearrange("(kc p) n -> p kc n", p=128),
                                       [128, KC, 256])
                    for q in range(2):
                        c = 2 * j + q
                        hd = c // 2
                        normalize_chunk(c)
                        bks = [main_bank(), main_bank()]
                        fns = []
                        for kc in range(KC):
                            for b in range(2):
                                fns.append(lambda e, kc=kc, b=b, q=q, sv=sv, bks=bks: e.matmul(
                                    psb[bks[b]][:, :], sv[:, kc, q * 128:(q + 1) * 128], hT[:, kc, b * 512:(b + 1) * 512],
                                    start=(kc == 0), stop=(kc == KC - 1)))
                        emit_group(PE, fns, reads=[bs] + B_h, writes=[B_ps[bks[0]], B_ps[bks[1]]])
                        for b in range(2):
                            emit(ACT, lambda e, b=b, bks=bks: e.activation(
                                out=ug[:, b, :], in_=psb[bks[b]][:, :], func=AF.Gelu_apprx_tanh),
                                reads=[B_ps[bks[b]]], writes=[B_ug[b]])
                        sbk = [main_bank(), main_bank()]
                        fns = []
                        for i in range(8):
                            o = psb[sbk[i // 4]][:, (i % 4) * 128:(i % 4 + 1) * 128]
                            fns.append(lambda e, o=o, hd=hd: e.matmul(o, ones_bf[:], bmat[:, hd, :], start=True, stop=False))
                            fns.append(lambda e, o=o, hd=hd, c=c, i=i: e.matmul(o, vq[:, c, i, :], wspT[:, hd, :],
                                                                                start=False, stop=True))
                        emit_group(PE, fns, reads=[B_vq[c], B_wsp, B_parg, B_cst], writes=[B_ps[sbk[0]], B_ps[sbk[1]]])
                        for b in range(2):
                            emit(DVE, lambda e, b=b, c=c, sbk=sbk: e.tensor_tensor(
                                out=ymix[:, c, b * 512:(b + 1) * 512], in0=psb[sbk[b]][:, :], in1=ug[:, b, :], op=ALU.mult),
                                reads=[B_ps[sbk[b]], B_ug[b]], writes=[B_vq[c]])
                B_ym = B_vq
                w_out_d = w_out_odd_d
            if stop == f"ymix{l}":
                dump_bf16(ymix, B_ym)
                return nc, finish()

            mix_end = [PE.last, ACT.last, DVE.last]
            for j in range(8):
                bs, sv = next_slot(w_out_d[:, 256 * j:256 * (j + 1)].rearrange("(kc p) n -> p kc n", p=128), [128, KC, 256])
                for q in range(2):
                    dc = 2 * j + q
                    bks = [main_bank(), main_bank()]
                    fns = []
                    for kc in range(KC):
                        for b in range(2):
                            fns.append(lambda e, kc=kc, b=b, q=q, sv=sv, bks=bks: e.matmul(
                                psb[bks[b]][:, :], sv[:, kc, q * 128:(q + 1) * 128], ymix[:, kc, b * 512:(b + 1) * 512],
                                start=(kc == 0), stop=(kc == KC - 1)))
                    emit_group(PE, fns, reads=[bs] + B_ym, writes=[B_ps[bks[0]], B_ps[bks[1]]])
                    for b in range(2):
                        emit(DVE, lambda e, b=b, dc=dc, bks=bks, l=l: e.scalar_tensor_tensor(
                            out=xT[:, dc, b * 512:(b + 1) * 512], in0=psb[bks[b]][:, :], scalar=mod(l, 2)[:, dc:dc + 1],
                            in1=xT[:, dc, b * 512:(b + 1) * 512], op0=ALU.mult, op1=ALU.add),
                            reads=[B_ps[bks[b]], B_mod[l], B_x[dc][b]], writes=[B_x[dc][b]])
                if l == 0:
                    ada_slots(0, 32 + j, 1)
            if stop == f"mix{l}":
                dump_f32(xT)
                return nc, finish()

            emit(DVE, lambda e, l=l: e.scalar_tensor_tensor(out=a2[:, l, :], in0=mod(l, 4), scalar=1.0, in1=nffn[:, l, :],
                                                            op0=ALU.add, op1=ALU.mult),
                 reads=[B_mod[l], B_par], writes=[B_mod[l]])
            for e_ in (ACT, DVE):
                for t_ in mix_end:
                    e_.wait(t_)
            B_rstd = rms_stats(f"n2_{l}")
            B_tmp = [Buf(f"tmpb{i}") for i in range(2)]
            B_hf = [Buf(f"hf{i}") for i in range(2)]
            lbk = [aux_bank(), aux_bank()]
            for c in range(KC):
                for b in range(2):
                    i = b
                    emit(DVE, lambda e, c=c, b=b, i=i, l=l: e.scalar_tensor_tensor(
                        out=tmpv[:, i, :], in0=xT[:, c, b * 512:(b + 1) * 512], scalar=a2[:, l, c:c + 1],
                        in1=rstd[:, b * 512:(b + 1) * 512], op0=ALU.mult, op1=ALU.mult),
                        reads=[B_x[c][b], B_rstd, B_mod[l]], writes=[B_tmp[i]])
                    emit(ACT, lambda e, c=c, b=b, i=i, l=l: e.activation(
                        out=hfv[:, i, :], in_=tmpv[:, i, :], func=AF.Identity, bias=mod(l, 3)[:, c:c + 1], scale=1.0),
                        reads=[B_tmp[i], B_mod[l]], writes=[B_hf[i]])
                    emit(DVE, lambda e, c=c, b=b, i=i: e.tensor_copy(out=hT[:, c, b * 512:(b + 1) * 512], in_=hfv[:, i, :]),
                         reads=[B_hf[i]], writes=[B_h[c]])
                    fns = [lambda e, c=c, b=b, i=i, t4=t4: e.matmul(
                        psb[lbk[0]][:, (4 * b + t4) * 16:(4 * b + t4 + 1) * 16], hfv[:, i, t4 * 128:(t4 + 1) * 128], wr[:, c, :],
                        start=(c == 0 and b == 0 and t4 == 0), stop=(c == KC - 1), skip_group_check=True) for t4 in range(4)]
                    emit_group(PE, fns, reads=[B_hf[i], B_par], writes=[B_ps[lbk[0]]])
            if stop == f"h2_{l}":
                dump_bf16(hT, B_h)
                return nc, finish()
            bk = lbk[0]
            aff = rt[:, 0:128]
            sel = rt[:, 128:256]
            eq1 = rt[:, 256:384]
            sel2 = rt[:, 384:512]
            ge2 = rt[:, 512:640]
            wts = rt[:, 640:768]
            comb = rt[:, 768:896]
            m1 = rt[:, 896:928]
            m2 = rt[:, 928:960]
            gs = rt[:, 960:992]
            gmax = rt[:, 992:1000]
            gmask = rt[:, 1000:1032]
            den = rt[:, 1032:1040]
            B_rt = Buf("rt")

            def g4(ap):
                return ap.rearrange("p (a k) -> p a k", k=4)

            def bl(ap, n):
                return ap.unsqueeze(2).to_broadcast([128, ap.shape[1], n])

            R = dict(reads=[B_rt], writes=[B_rt])
            emit(ACT, lambda e, bk=bk: e.activation(out=aff, in_=psb[bk][:, 0:128], func=AF.Sigmoid), reads=[B_ps[bk]], writes=[B_rt])
            emit(DVE, lambda e: e.tensor_tensor(out=sel.rearrange("p (i x) -> p i x", i=8), in0=aff.rearrange("p (i x) -> p i x", i=8),
                                                in1=rbias[:].unsqueeze(1).to_broadcast([128, 8, NE]), op=ALU.add),
                 reads=[B_rt, B_par], writes=[B_rt])
            emit(DVE, lambda e: e.tensor_reduce(out=m1, in_=g4(sel), axis=AX.X, op=ALU.max), **R)
            emit(DVE, lambda e: e.tensor_tensor(out=g4(eq1), in0=g4(sel), in1=bl(m1, 4), op=ALU.is_equal), **R)
            emit(DVE, lambda e: e.scalar_tensor_tensor(out=sel2, in0=eq1, scalar=-1e30, in1=sel, op0=ALU.mult, op1=ALU.add), **R)
            emit(DVE, lambda e: e.tensor_reduce(out=m2, in_=g4(sel2), axis=AX.X, op=ALU.max), **R)
            emit(DVE, lambda e: e.tensor_tensor(out=gs, in0=m1, in1=m2, op=ALU.add), **R)
            emit(DVE, lambda e: e.tensor_reduce(out=gmax, in_=g4(gs), axis=AX.X, op=ALU.max), **R)
            emit(DVE, lambda e: e.tensor_tensor(out=g4(gmask), in0=g4(gs), in1=bl(gmax, 4), op=ALU.is_equal), **R)
            emit(DVE, lambda e: e.tensor_tensor(out=g4(ge2), in0=g4(sel), in1=bl(m2, 4), op=ALU.is_ge), **R)
            emit(DVE, lambda e: e.tensor_tensor(out=g4(ge2), in0=g4(ge2), in1=bl(gmask, 4), op=ALU.mult), **R)
            emit(DVE, lambda e: e.tensor_tensor(out=wts, in0=aff, in1=ge2, op=ALU.mult), **R)
            emit(DVE, lambda e: e.tensor_reduce(out=den, in_=wts.rearrange("p (i x) -> p i x", i=8), axis=AX.X, op=ALU.add), **R)
            emit(DVE, lambda e: e.reciprocal(out=den, in_=den), **R)
            emit(DVE, lambda e: e.tensor_tensor(out=comb.rearrange("p (i x) -> p i x", i=8), in0=wts.rearrange("p (i x) -> p i x", i=8),
                                                in1=bl(den, NE), op=ALU.mult), **R)
            hib = arb[:, 0:128].rearrange("p (i x) -> p i x", i=8)
            chl = rt[:, 256:512].rearrange("p (i x) -> p i x", i=8)
            comb3 = comb.rearrange("p (i x) -> p i x", i=8)
            emit(DVE, lambda e: e.tensor_copy(out=hib, in_=comb3), **R)
            emit(DVE, lambda e: e.tensor_copy(out=chl[:, :, 0:16], in_=hib), **R)
            emit(DVE, lambda e: e.tensor_tensor(out=chl[:, :, 16:32], in0=comb3, in1=chl[:, :, 0:16], op=ALU.subtract), **R)
            bkc = [aux_bank(), aux_bank()]
            fns = [lambda e, i=i: e.transpose(psb[bkc[i // 4]][0:32, (i % 4) * 128:(i % 4 + 1) * 128], chl[:, i, :], ident[:])
                   for i in range(8)]
            emit_group(PE, fns, reads=[B_rt, B_par], writes=[B_ps[bkc[0]], B_ps[bkc[1]]])
            for b in range(2):
                emit(ACT, lambda e, b=b: e.activation(out=combT[:, b * 512:(b + 1) * 512], in_=psb[bkc[b]][0:32, :], func=AF.Copy),
                     reads=[B_ps[bkc[b]]], writes=[B_combT])
            if stop == f"comb{l}":
                barrier()
                emit(DVE, lambda e: e.memset(xT[:, 0, :], 0.0), writes=[B_x[0][0], B_x[0][1]])
                emit(DVE, lambda e: e.tensor_copy(out=xT[0:32, 0, :], in_=combT[:]), reads=[B_combT], writes=[B_x[0][0], B_x[0][1]])
                dump_f32(xT)
                return nc, finish()
            barrier()

            aT = arb[:, 0:8192].rearrange("p (u f t) -> p u f t", u=2, f=4)
            sg = arf[:, 2048:3072].rearrange("p (u t) -> p u t", u=2)
            cb = arf[:, 0:2048].rearrange("p (u t) -> p u t", u=2)
            B_aT = [[Buf(f"aT{u}_{f}") for f in range(4)] for u in range(2)]
            B_sg = [Buf("sg0"), Buf("sg1")]
            B_cb = [Buf("cb0"), Buf("cb1")]
            B_sel = [Buf("sel0"), Buf("sel1")]
            nxt_ada = ([(0, j) for j in range(40, 48)] + [(1, j) for j in range(48)]) if l == 0 else []
            set_ring(RING + 2, gate_toks=[PE.last, ACT.last, DVE.last])
            sgi = [0]

            def gate_up(ex):
                u = ex % 2
                bks = [aux_bank(), aux_bank()]
                emit(DVE, lambda e, ex=ex, u=u: e.tensor_tensor(out=selmat[:, u, :], in0=ident[0:32, ex:ex + 1].to_broadcast([32, 128]),
                                                                in1=ident[0:32, 16 + ex:17 + ex].to_broadcast([32, 128]), op=ALU.add),
                     reads=[B_par], writes=[B_sel[u]])
                fns = [lambda e, b=b, u=u, bks=bks: e.matmul(psb[bks[b]][:, :], selmat[:, u, :], combT[:, b * 512:(b + 1) * 512],
                                                             start=True, stop=True) for b in range(2)]
                emit_group(PE, fns, reads=[B_combT, B_sel[u]], writes=[B_ps[bks[0]], B_ps[bks[1]]])
                for b in range(2):
                    emit(ACT, lambda e, b=b, u=u, bks=bks: e.activation(out=cb[:, u, b * 512:(b + 1) * 512], in_=psb[bks[b]][:, :],
                                                                        func=AF.Copy),
                         reads=[B_ps[bks[b]]], writes=[B_cb[u]])
                for half in range(2):
                    gs_ = next_slot(wg_d[l, ex][:, 256 * half:256 * (half + 1)].rearrange("(kc p) n -> p kc n", p=128), [128, KC, 256])
                    us_ = next_slot(wu_d[l, ex][:, 256 * half:256 * (half + 1)].rearrange("(kc p) n -> p kc n", p=128), [128, KC, 256])
                    for q in range(2):
                        fc = 2 * half + q
                        for b in range(2):
                            bg, bu = main_bank(), main_bank()
                            fns = []
                            for kc in range(KC):
                                fns.append((lambda e, kc=kc, q=q, b=b, bg=bg, sv=gs_[1]: e.matmul(
                                    psb[bg][:, :], sv[:, kc, q * 128:(q + 1) * 128], hT[:, kc, b * 512:(b + 1) * 512],
                                    start=(kc == 0), stop=(kc == KC - 1)), [B_h[kc]]))
                            for kc in range(KC):
                                fns.append(lambda e, kc=kc, q=q, b=b, bu=bu, sv=us_[1]: e.matmul(
                                    psb[bu][:, :], sv[:, kc, q * 128:(q + 1) * 128], hT[:, kc, b * 512:(b + 1) * 512],
                                    start=(kc == 0), stop=(kc == KC - 1)))
                            emit_group(PE, fns, reads=[gs_[0], us_[0]], writes=[B_ps[bg], B_ps[bu]])
                            si = sgi[0]
                            sgi[0] = 1 - si
                            emit(ACT, lambda e, bg=bg, si=si: e.activation(out=sg[:, si, :], in_=psb[bg][:, :], func=AF.Silu),
                                 reads=[B_ps[bg]], writes=[B_sg[si]])
                            emit(DVE, lambda e, si=si, u=u, b=b: e.tensor_tensor(
                                out=sg[:, si, :], in0=sg[:, si, :], in1=cb[:, u, b * 512:(b + 1) * 512], op=ALU.mult),
                                reads=[B_sg[si], B_cb[u]], writes=[B_sg[si]])
                            emit(DVE, lambda e, si=si, u=u, b=b, fc=fc, bu=bu: e.tensor_tensor(
                                out=aT[:, u, fc, b * 512:(b + 1) * 512], in0=psb[bu][:, :], in1=sg[:, si, :], op=ALU.mult),
                                reads=[B_ps[bu], B_sg[si]], writes=[B_aT[u][fc]])

            def down(ex):
                u = ex % 2
                for half in range(2):
                    ds_ = next_slot(wd_d[l, ex][:, 1024 * half:1024 * (half + 1)].rearrange("(kc p) n -> p kc n", p=128), [128, 4, 1024])
                    for q in range(8):
                        dc = 8 * half + q
                        for b in range(2):
                            bk = main_bank()
                            fns = [lambda e, kc=kc, q=q, b=b, bk=bk, sv=ds_[1]: e.matmul(
                                psb[bk][:, :], sv[:, kc, q * 128:(q + 1) * 128], aT[:, u, kc, b * 512:(b + 1) * 512],
                                start=(kc == 0), stop=(kc == 3)) for kc in range(4)]
                            emit_group(PE, fns, reads=[ds_[0]] + B_aT[u], writes=[B_ps[bk]])
                            emit(DVE, lambda e, b=b, dc=dc, bk=bk, l=l: e.scalar_tensor_tensor(
                                out=xT[:, dc, b * 512:(b + 1) * 512], in0=psb[bk][:, :], scalar=mod(l, 5)[:, dc:dc + 1],
                                in1=xT[:, dc, b * 512:(b + 1) * 512], op0=ALU.mult, op1=ALU.add),
                                reads=[B_ps[bk], B_mod[l], B_x[dc][b]], writes=[B_x[dc][b]])

            nex = NE
            for ex in range(nex):
                gate_up(ex)
                if l == 0 and ex <= 1:
                    for _ in range(4):
                        l_, j_ = nxt_ada.pop(0)
                        ada_slots(l_, j_, 1)
                if ex >= 1:
                    down(ex - 1)
                if ex >= 2:
                    for _ in range(4):
                        if nxt_ada:
                            l_, j_ = nxt_ada.pop(0)
                            ada_slots(l_, j_, 1)
            down(nex - 1)
            while nxt_ada:
                l_, j_ = nxt_ada.pop(0)
                ada_slots(l_, j_, 1)
            set_ring(RING)
            if stop == f"moe{l}":
                dump_f32(xT)
                return nc, finish()

        rstd_fin = rt[:, 0:1024]
        B_rstd = rms_stats("fin", rd=rstd_fin)
        for c in range(KC):
            for b in range(2):
                emit(DVE, lambda e, c=c, b=b: e.scalar_tensor_tensor(
                    out=xT[:, c, b * 512:(b + 1) * 512], in0=xT[:, c, b * 512:(b + 1) * 512], scalar=fnorm[:, c:c + 1],
                    in1=rstd_fin[:, b * 512:(b + 1) * 512], op0=ALU.mult, op1=ALU.mult),
                    reads=[B_x[c][b], B_rstd, B_par], writes=[B_x[c][b]])
            emit(SP, lambda e, c=c: e.dma_start(out=outT_d[:, c, :], in_=xT[:, c, :]),
                 reads=[B_x[c][0], B_x[c][1]], dsem=out_sem)
        return nc, finish()


def _fm(v):
    v = np.asarray(v, np.float32)
    lead = v.shape[:-1]
    n = v.shape[-1] // 128
    return np.ascontiguousarray(np.moveaxis(v.reshape(*lead, n, 128), -1, 0))


def make_in_maps(inp):
    f = lambda a: np.ascontiguousarray(np.asarray(a, np.float32))
    x = f(inp["x"])
    shared = {
        "ada_w": f(inp["ada_w"]),
        "ada_bT": _fm(inp["ada_b"]),
        "nmixT": _fm(inp["norm_mix"]),
        "nffnT": _fm(inp["norm_ffn"]),
        "fnormT": _fm(inp["final_norm"]),
        "w_in_even": f(inp["w_in_even"][0]),
        "w_pool": f(inp["w_pool"][0]),
        "pscaleT": _fm(inp["pool_scale"][0]),
        "convwT": np.ascontiguousarray(np.transpose(f(inp["conv_w"][0]), (2, 0, 1))),
        "w_out_even": f(inp["w_out_even"][0]),
        "w_in_odd": f(inp["w_in_odd"][0]),
        "sgu_norm": f(inp["sgu_norm"]).reshape(1, D),
        "w_spatial": f(inp["w_spatial"][0]),
        "b_spatial": f(inp["b_spatial"][0]).reshape(1, 8 * 128),
        "w_out_odd": f(inp["w_out_odd"][0]),
        "w_router": f(inp["w_router"]),
        "router_bias": f(inp["router_bias"]).reshape(1, NE),
        "w_gate": f(inp["w_gate"]),
        "w_up": f(inp["w_up"]),
        "w_down": f(inp["w_down"]),
        "ident": np.eye(128, dtype=np.float32),
        "trilT": np.triu(np.ones((128, 128), np.float32)),
    }
    maps = []
    for core in range(NCORES):
        b, half = core // 2, core % 2
        t0 = half * NT
        xs = x[b, t0:t0 + NT]
        xT = np.ascontiguousarray(np.transpose(xs.reshape(NT, KC, 128), (2, 1, 0)))
        if half == 1:
            xh = x[b, t0 - HALO:t0]
        else:
            xh = np.zeros((HALO, D), np.float32)
        xhT = np.ascontiguousarray(np.transpose(xh.reshape(HALO, KC, 128), (2, 1, 0)))
        pcv = np.zeros((128, 65), np.float32)
        pcv[:, 0] = float(half)
        for g, w in enumerate((2, 4, 8, 16)):
            pos = np.arange(t0 + 1, t0 + 17, dtype=np.float32)
            pcv[:, 1 + 16 * g:17 + 16 * g] = (1.0 / np.minimum(pos, float(w)))[None, :]
        m = dict(shared)
        m["xT"] = xT
        m["xhT"] = xhT
        m["cT"] = _fm(inp["c"][b])
        m["pc"] = pcv
        maps.append(m)
    return maps


_NC_CACHE = {}


def kernel(**inputs):
    if "nc" not in _NC_CACHE:
        _NC_CACHE["nc"] = build()[0]
    nc = _NC_CACHE["nc"]
    maps = make_in_maps(inputs)
    res = run_bass_kernel_spmd(nc, maps, core_ids=list(range(NCORES)))
    out = np.empty((4, 2 * NT, D), np.float32)
    for core in range(NCORES):
        b, half = core // 2, core % 2
        oT = np.asarray(res.results[core]["outT"])
        out[b, half * NT:(half + 1) * NT] = np.transpose(oT, (2, 1, 0)).reshape(NT, D)
    return out
```

```python
import numpy as np
from contextlib import ExitStack
import concourse.bass as bass
import concourse.mybir as mybir
from concourse.bass_utils import run_bass_kernel_spmd

F32 = mybir.dt.float32
BF16 = mybir.dt.bfloat16
AF = mybir.ActivationFunctionType
ALU = mybir.AluOpType
AX = mybir.AxisListType

D = 2048
NT = 1024
KC = 16
HALO = 16
NE = 16
EPS = 1e-6
RING = 5
SLOT = 4096
NCORES = 8


class Eng:
    def __init__(self, name, nc, stack, nsem, limit=4000):
        self.name = name
        self.ops = []
        self.sems = [stack.enter_context(nc.semaphore(f"s_{name}_{i}")) for i in range(nsem)]
        self.cur = 0
        self.count = 0
        self.limit = limit
        self.waited = {}
        self.last = None

    def new_token(self):
        if self.count >= self.limit:
            self.cur += 1
            self.count = 0
        self.count += 1
        tok = (self.sems[self.cur], self.count, (self.name, self.cur), 1)
        self.last = tok
        return tok

    def wait(self, tok):
        if tok is None:
            return
        key = tok[2]
        if self.waited.get(key, 0) >= tok[1]:
            return
        self.waited[key] = tok[1]
        self.ops.append(("w", tok[0], tok[1]))


class DmaSem:
    def __init__(self, name, nc, stack):
        self.name = name
        self.sem = stack.enter_context(nc.semaphore(f"d_{name}"))
        self.count = 0

    def new_token(self):
        self.count += 1
        return (self.sem, 16 * self.count, ("dma", self.name), 16)


class Buf:
    def __init__(self, name, const=False):
        self.name = name
        self.w = None
        self.r = {}
        self.const = const


def _deps(E, reads, writes):
    for b in reads:
        if b.w is not None and not (E.name == "pe" and b.w[2][0] == "pe"):
            E.wait(b.w)
    for b in writes:
        if b.w is not None and not (E.name == "pe" and b.w[2][0] == "pe"):
            E.wait(b.w)
        for t in b.r.values():
            if not (E.name == "pe" and t[2][0] == "pe"):
                E.wait(t)


def _commit(tok, reads, writes, rkey):
    for b in reads:
        if not b.const:
            b.r[rkey] = tok
    for b in writes:
        b.w = tok
        b.r = {}


def emit(E, fn, reads=(), writes=(), dsem=None):
    _deps(E, reads, writes)
    if dsem is not None:
        tok = dsem.new_token()
        rkey = ("dma", dsem.name, dsem.count)
    else:
        tok = E.new_token()
        rkey = E.name
    E.ops.append(("i", fn, tok))
    _commit(tok, reads, writes, rkey)
    return tok


def emit_group(E, fns, reads=(), writes=()):
    _deps(E, reads, writes)
    tok = E.new_token()
    allreads = list(reads)
    for i, f in enumerate(fns):
        if isinstance(f, tuple):
            fn, rds = f
            _deps(E, rds, ())
            for r in rds:
                if r not in allreads:
                    allreads.append(r)
        else:
            fn = f
        E.ops.append(("i", fn, tok if i == len(fns) - 1 else None))
    _commit(tok, allreads, writes, E.name)
    return tok


def replay(E, h):
    for op in E.ops:
        if op[0] == "w":
            h.wait_ge(op[1], op[2])
        else:
            inst = op[1](h)
            if op[2] is not None:
                inst.then_inc(op[2][0], op[2][3])


def build(stop=None):
    nc = bass.Bass("TRN2", target_bir_lowering=False)

    def din(name, shape):
        return nc.dram_tensor(name, list(shape), F32, kind="ExternalInput").ap()

    xT_d = din("xT", [128, KC, NT])
    xhT_d = din("xhT", [128, KC, HALO])
    cT_d = din("cT", [128, KC])
    pc_d = din("pc", [128, 65])
    ada_w_d = din("ada_w", [2, 48, 128, SLOT])
    ada_bT_d = din("ada_bT", [128, 2, 96])
    nmix_d = din("nmixT", [128, 2, KC])
    nffn_d = din("nffnT", [128, 2, KC])
    fnorm_d = din("fnormT", [128, KC])
    w_in_even_d = din("w_in_even", [16, 128, SLOT])
    w_pool_d = din("w_pool", [4, 256, 256])
    pscale_d = din("pscaleT", [128, 8])
    convw_d = din("convwT", [128, 3, 8])
    w_out_even_d = din("w_out_even", [8, 128, SLOT])
    w_in_odd_d = din("w_in_odd", [16, 128, SLOT])
    sgn_d = din("sgu_norm", [1, D])
    wsp_d = din("w_spatial", [8, 128, 128])
    bsp_d = din("b_spatial", [1, 8 * 128])
    w_out_odd_d = din("w_out_odd", [8, 128, SLOT])
    wr_d = din("w_router", [D, NE])
    rb_d = din("router_bias", [1, NE])
    wg_d = din("w_gate", [2, NE, 2, 128, SLOT])
    wu_d = din("w_up", [2, NE, 2, 128, SLOT])
    wd_d = din("w_down", [2, NE, 2, 128, SLOT])
    ident_d = din("ident", [128, 128])
    tril_d = din("trilT", [128, 128])
    outT_d = nc.dram_tensor("outT", [128, KC, NT], F32, kind="ExternalOutput").ap()

    with ExitStack() as st:
        def sb(name, shape, dt):
            return st.enter_context(nc.sbuf_tensor("sb_" + name, list(shape), dt))

        xT = sb("xT", [128, KC, NT], F32)
        hT = sb("hT", [128, KC, NT], BF16)
        xh = sb("xh", [128, KC, HALO], F32)
        hh = sb("hh", [128, KC, HALO], BF16)
        ring = sb("ring", [128, RING, SLOT], BF16)
        arb = sb("arb", [128, 18432], BF16)
        arf = sb("arf", [128, 3136], F32)
        wspn = arf[:, 0:1024].rearrange("p (h s) -> p h s", h=8)
        cT = sb("cT", [128, KC], F32)
        cact = sb("cact", [128, KC], BF16)
        pc = sb("pc", [128, 65], F32)
        ada_bT = sb("ada_bT", [128, 2, 96], F32)
        modsb = sb("modsb", [128, 2, 96], F32)
        nmix = sb("nmix", [128, 2, KC], F32)
        nffn = sb("nffn", [128, 2, KC], F32)
        fnorm = sb("fnorm", [128, KC], F32)
        a1 = sb("a1", [128, 2, KC], F32)
        a2 = sb("a2", [128, 2, KC], F32)
        pscale = sb("pscale", [128, 8], F32)
        convw = sb("convw", [128, 3, 8], F32)
        wpool = sb("wpool", [128, 4, 2, 256], BF16)
        wspT = sb("wspT", [128, 8, 128], BF16)
        bmat = sb("bmat", [128, 8, 128], BF16)
        wr = sb("wr", [128, KC, NE], F32)
        rbias = sb("rbias", [128, NE], F32)
        ident = sb("ident", [128, 128], F32)
        tril = sb("tril", [128, 128], F32)
        ones_bf = sb("ones_bf", [128, 128], BF16)
        selmat = sb("selmat", [32, 2, 128], BF16)
        combT = sb("combThl", [32, 1024], BF16)
        epst = sb("epst", [128, 1], F32)
        rt = sb("rt", [128, 1040], F32)
        small = sb("small", [128, 64], F32)

        psb = [st.enter_context(nc.psum_tensor(f"ps{i}", [128, 512], F32)) for i in range(8)]

        PE = Eng("pe", nc, st, 6)
        ACT = Eng("act", nc, st, 4)
        DVE = Eng("dve", nc, st, 6)
        POOL = Eng("pool", nc, st, 2)
        SP = Eng("sp", nc, st, 2)
        slot_sems = [DmaSem(f"slot{i}", nc, st) for i in range(RING + 2)]
        par_sem = DmaSem("par", nc, st)
        parg_sem = DmaSem("parg", nc, st)
        x_sem = DmaSem("x", nc, st)
        misc_sem = DmaSem("misc", nc, st)
        out_sem = DmaSem("out", nc, st)

        block = st.enter_context(nc.Block())

        B_x = [[Buf(f"x{c}_{b}") for b in range(2)] for c in range(KC)]
        B_h = [Buf(f"h{c}") for c in range(KC)]
        B_hh = Buf("hh")
        B_slot = [Buf(f"slot{i}") for i in range(RING + 2)]
        B_ps = [Buf(f"ps{i}") for i in range(8)]
        B_par = Buf("par", const=True)
        B_mod = [Buf(f"mod{l}") for l in range(2)]
        state = {"main": 0, "aux": 0, "pos": 0, "ring": list(range(RING)), "gate": {}}

        def main_bank():
            i = state["main"]
            state["main"] = (i + 1) % 6
            return i

        def aux_bank():
            i = 6 + state["aux"]
            state["aux"] = (state["aux"] + 1) % 2
            return i

        def barrier():
            lasts = [PE.last, ACT.last, DVE.last]
            for e in (ACT, DVE):
                for t in lasts:
                    if t is not None:
                        e.wait(t)

        def set_ring(n, gate_toks=None):
            state["ring"] = list(range(n))
            state["pos"] = 0
            state["gate"] = {i: gate_toks for i in range(RING, n)} if gate_toks else {}

        def next_slot(src_ap, view):
            rl = state["ring"]
            i = rl[state["pos"] % len(rl)]
            state["pos"] += 1
            nelem = 1
            for s_ in view[1:]:
                nelem *= s_
            if i < RING:
                dst = ring[:, i, 0:nelem]
            else:
                o = 8192 + (i - RING) * SLOT
                dst = arb[:, o:o + nelem]
                if i in state["gate"]:
                    for t_ in state["gate"].pop(i):
                        POOL.wait(t_)
            if len(view) == 3:
                dst = dst.rearrange("p (a b) -> p a b", a=view[1])
            assert nelem == SLOT
            flat = ring[:, i, 0:SLOT] if i < RING else arb[:, 8192 + (i - RING) * SLOT:8192 + (i - RING + 1) * SLOT]
            emit(POOL, lambda e, flat=flat, src=src_ap: e.dma_start(out=flat.rearrange("p (a b) -> p a b", b=1024),
                                                                    in_=src.rearrange("p (a b) -> p a b", b=1024)),
                 reads=[], writes=[B_slot[i]], dsem=slot_sems[i])
            return B_slot[i], dst

        def spload(dst, src, sem=par_sem):
            emit(SP, lambda e, dst=dst, src=src: e.dma_start(out=dst, in_=src), dsem=sem)

        for c in range(KC):
            emit(SP, lambda e, c=c: e.dma_start(out=xT[:, c, :], in_=xT_d[:, c, :]),
                 writes=[B_x[c][0], B_x[c][1]], dsem=x_sem)
        xtok = (x_sem.sem, 16 * x_sem.count, ("dma", "x"), 16)
        for c in range(KC):
            for b in range(2):
                B_x[c][b].w = xtok
        spload(xh[:], xhT_d)
        spload(cT[:], cT_d)
        spload(pc[:], pc_d)
        spload(ada_bT[:], ada_bT_d)
        spload(nmix[:], nmix_d)
        spload(nffn[:], nffn_d)
        spload(fnorm[:], fnorm_d)
        spload(pscale[:], pscale_d)
        spload(convw[:], convw_d)
        spload(wspn, wsp_d.rearrange("h t s -> t h s"))
        spload(wr[:], wr_d.rearrange("(kc p) e -> p kc e", p=128))
        spload(rbias[:], rb_d[0, :].partition_broadcast(128))
        spload(ident[:], ident_d)
        spload(tril[:], tril_d)
        B_par.w = (par_sem.sem, 16 * par_sem.count, ("dma", "par"), 16)

        B_cst = Buf("cst")
        emit(DVE, lambda e: e.memset(ones_bf[:], 1.0), writes=[B_cst])
        emit(DVE, lambda e: e.memset(epst[:], EPS), writes=[B_cst])
        emit(DVE, lambda e: e.memset(bmat[:], 0.0), writes=[B_cst])
        emit(POOL, lambda e: e.dma_start(out=wpool[:], in_=w_pool_d.rearrange("g (kc p) n -> p g kc n", p=128)),
             dsem=parg_sem)
        emit(POOL, lambda e: e.dma_start(out=bmat[0:1, :, :], in_=bsp_d.rearrange("o (h t) -> o h t", h=8)),
             reads=[B_cst], dsem=parg_sem)
        B_parg = Buf("parg", const=True)
        B_parg.w = (parg_sem.sem, 16 * parg_sem.count, ("dma", "parg"), 16)
        B_cst.const = True

        emit(ACT, lambda e: e.activation(out=cact[:], in_=cT[:], func=AF.Silu), reads=[B_par], writes=[B_cst])

        B_wsp = Buf("wsp")
        for half in range(2):
            bk = aux_bank()
            fns = []
            for q in range(4):
                hd = half * 4 + q
                fns.append(lambda e, hd=hd, q=q, bk=bk: e.transpose(psb[bk][:, q * 128:(q + 1) * 128], wspn[:, hd, :], ident[:]))
            emit_group(PE, fns, reads=[B_par], writes=[B_ps[bk]])
            emit(DVE, lambda e, half=half, bk=bk: e.tensor_tensor(
                out=wspT[:, half * 4:(half + 1) * 4, :],
                in0=psb[bk][:].rearrange("p (q t) -> p q t", q=4),
                in1=tril[:].unsqueeze(1).to_broadcast([128, 4, 128]), op=ALU.mult),
                reads=[B_ps[bk], B_par], writes=[B_wsp])

        def ada_slots(l, j0, nslots):
            for s in range(j0, j0 + nslots):
                src = ada_w_d[l, s]
                bs, sv = next_slot(src, [128, KC, 256])
                bk = aux_bank()
                fns = []
                for q in range(2):
                    for kc in range(KC):
                        fns.append(lambda e, q=q, kc=kc, sv=sv, bk=bk: e.matmul(
                            psb[bk][:, q:q + 1], sv[:, kc, q * 128:(q + 1) * 128], cact[:, kc:kc + 1],
                            start=(kc == 0), stop=(kc == KC - 1)))
                emit_group(PE, fns, reads=[bs, B_cst], writes=[B_ps[bk]])
                emit(DVE, lambda e, s=s, bk=bk, l=l: e.tensor_tensor(
                    out=modsb[:, l, 2 * s:2 * s + 2], in0=psb[bk][:, 0:2], in1=ada_bT[:, l, 2 * s:2 * s + 2], op=ALU.add),
                    reads=[B_ps[bk], B_par], writes=[B_mod[l]])

        def mod(l, k):
            return modsb[:, l, 16 * k:16 * (k + 1)]

        sqv = arb[:, 16384:18432].rearrange("p (b t) -> p b t", b=2)
        rstd = arf[:, 0:1024]
        tmpv = arf[:, 1024:2048].rearrange("p (b t) -> p b t", b=2)
        hfv = arf[:, 2048:3072].rearrange("p (b t) -> p b t", b=2)
        lgT = rt[0:16, 0:1024]
        B_combT = Buf("combT")

        def rms_stats(tag, rd=None):
            if rd is None:
                rd = rstd
            B_sq = [Buf(f"sq{tag}{i}") for i in range(2)]
            B_rstd = Buf(f"rstd{tag}")
            bk0, bk1 = aux_bank(), aux_bank()
            bks = [bk0, bk1]
            for c in range(KC):
                i = c % 2
                if c % 4 < 2:
                    emit(ACT, lambda e, c=c, i=i: e.activation(out=sqv[:, i, :], in_=xT[:, c, :], func=AF.Square),
                         reads=[B_x[c][0], B_x[c][1]], writes=[B_sq[i]])
                else:
                    emit(DVE, lambda e, c=c, i=i: e.tensor_tensor(out=sqv[:, i, :], in0=xT[:, c, :], in1=xT[:, c, :], op=ALU.mult),
                         reads=[B_x[c][0], B_x[c][1]], writes=[B_sq[i]])
                fns = []
                for b in range(2):
                    fns.append(lambda e, b=b, i=i, c=c: e.matmul(
                        psb[bks[b]][:, :], ones_bf[:], sqv[:, i, b * 512:(b + 1) * 512],
                        start=(c == 0), stop=(c == KC - 1)))
                emit_group(PE, fns, reads=[B_sq[i], B_cst], writes=[B_ps[bk0], B_ps[bk1]])
            for b in range(2):
                emit(ACT, lambda e, b=b: e.activation(out=rd[:, b * 512:(b + 1) * 512], in_=psb[bks[b]][:, :],
                                                     func=AF.Ln, scale=1.0 / D, bias=epst[:, 0:1]),
                     reads=[B_ps[bks[b]], B_cst], writes=[B_rstd])
                emit(ACT, lambda e, b=b: e.activation(out=rd[:, b * 512:(b + 1) * 512], in_=rd[:, b * 512:(b + 1) * 512],
                                                     func=AF.Exp, scale=-0.5),
                     reads=[B_rstd], writes=[B_rstd])
            return B_rstd

        def dump_f32(src3):
            for c in range(KC):
                emit(SP, lambda e, c=c: e.dma_start(out=outT_d[:, c, :], in_=src3[:, c, :]),
                     reads=[B_x[c][0], B_x[c][1]], dsem=out_sem)

        def finish():
            SP.ops.append(("w", out_sem.sem, 16 * out_sem.count))

            @block.tensor
            def _(h):
                replay(PE, h)

            @block.scalar
            def _(h):
                replay(ACT, h)

            @block.vector
            def _(h):
                replay(DVE, h)

            @block.gpsimd
            def _(h):
                replay(POOL, h)

            @block.sync
            def _(h):
                replay(SP, h)

        def dump_bf16(src3, bufs):
            barrier()
            for c in range(KC):
                emit(DVE, lambda e, c=c: e.tensor_copy(out=xT[:, c, :], in_=src3[:, c, :]),
                     reads=bufs, writes=[B_x[c][0], B_x[c][1]])
            dump_f32(xT)

        B_hs = Buf("hs")
        sqh = arb[:, 0:256].rearrange("p (c t) -> p c t", c=KC)
        rsh = small[:, 0:16]
        th = rt[:, 0:256].rearrange("p (c t) -> p c t", c=KC)
        B_rstd0 = rms_stats("n1_0")
        emit(ACT, lambda e: e.activation(out=sqh, in_=xh[:], func=AF.Square), reads=[B_par], writes=[B_hs])
        bk = aux_bank()
        fns = [lambda e, c=c, bk=bk: e.matmul(psb[bk][:, 0:HALO], ones_bf[:], sqh[:, c, :],
                                              start=(c == 0), stop=(c == KC - 1)) for c in range(KC)]
        emit_group(PE, fns, reads=[B_hs, B_cst], writes=[B_ps[bk]])
        emit(ACT, lambda e, bk=bk: e.activation(out=rsh, in_=psb[bk][:, 0:HALO], func=AF.Sqrt,
                                                scale=1.0 / D, bias=epst[:, 0:1]),
             reads=[B_ps[bk], B_cst], writes=[B_hs])
        emit(DVE, lambda e: e.reciprocal(out=rsh, in_=rsh), reads=[B_hs], writes=[B_hs])
        emit(DVE, lambda e: e.tensor_tensor(out=th, in0=xh[:], in1=rsh.unsqueeze(1).to_broadcast([128, KC, HALO]),
                                            op=ALU.mult), reads=[B_hs, B_par], writes=[B_hs])
        ada_slots(0, 0, 16)
        for l in range(2):
            emit(DVE, lambda e, l=l: e.scalar_tensor_tensor(out=a1[:, l, :], in0=mod(l, 1), scalar=1.0, in1=nmix[:, l, :],
                                                            op0=ALU.add, op1=ALU.mult),
                 reads=[B_mod[l], B_par], writes=[B_mod[l]])
            if l == 0:
                B_rstd = B_rstd0
                emit(DVE, lambda e: e.tensor_tensor(out=th, in0=th, in1=a1[:, 0, :].unsqueeze(2).to_broadcast([128, KC, HALO]),
                                                    op=ALU.mult), reads=[B_hs, B_mod[0]], writes=[B_hs])
                emit(DVE, lambda e: e.tensor_tensor(out=hh[:], in0=th, in1=mod(0, 0).unsqueeze(2).to_broadcast([128, KC, HALO]),
                                                    op=ALU.add), reads=[B_hs, B_mod[0]], writes=[B_hh])
            else:
                barrier()
                B_rstd = rms_stats(f"n1_{l}")
            B_tmp = [Buf(f"tmp{i}") for i in range(2)]
            for c in range(KC):
                for b in range(2):
                    i = b
                    emit(DVE, lambda e, c=c, b=b, i=i, l=l: e.scalar_tensor_tensor(
                        out=tmpv[:, i, :], in0=xT[:, c, b * 512:(b + 1) * 512], scalar=a1[:, l, c:c + 1],
                        in1=rstd[:, b * 512:(b + 1) * 512], op0=ALU.mult, op1=ALU.mult),
                        reads=[B_x[c][b], B_rstd, B_mod[l]], writes=[B_tmp[i]])
                    emit(ACT, lambda e, c=c, b=b, i=i, l=l: e.activation(
                        out=hT[:, c, b * 512:(b + 1) * 512], in_=tmpv[:, i, :], func=AF.Identity,
                        bias=mod(l, 0)[:, c:c + 1], scale=1.0),
                        reads=[B_tmp[i], B_mod[l]], writes=[B_h[c]])
            if stop == f"h1_{l}":
                dump_bf16(hT, B_h)
                return nc, finish()
            barrier()

            ymix = arb[:, 0:16384].rearrange("p (c t) -> p c t", c=KC)
            B_ym = [Buf(f"ym{c}") for c in range(KC)]
            if l == 0:
                zb = arf[:, 0:1040]
                sA = arf[:, 1040:2080]
                sB = arf[:, 2080:3120]
                t16 = arf[:, 3120:3136]
                B_zb, B_sA, B_sB, B_t16 = Buf("zb"), Buf("sA"), Buf("sB"), Buf("t16")
                pooled = arb[:, 16384:18432].rearrange("p (c t) -> p c t", c=2)
                B_pl = [Buf("pl0"), Buf("pl1")]
                flag = pc[:, 0:1]

                def zgroup(sv, q, with_halo):
                    bks = [main_bank(), main_bank()]
                    fns = []
                    for kc in range(KC):
                        for b in range(2):
                            fns.append((lambda e, kc=kc, b=b, bks=bks: e.matmul(
                                psb[bks[b]][:, :], sv[:, kc, q * 128:(q + 1) * 128], hT[:, kc, b * 512:(b + 1) * 512],
                                start=(kc == 0), stop=(kc == KC - 1)), [B_h[kc]]))
                    bh = None
                    if with_halo:
                        bh = aux_bank()
                        for kc in range(KC):
                            fns.append(lambda e, kc=kc, bh=bh: e.matmul(
                                psb[bh][:, 0:HALO], sv[:, kc, q * 128:(q + 1) * 128], hh[:, kc, :],
                                start=(kc == 0), stop=(kc == KC - 1)))
                    return bks, bh, fns

                mod_sched = list(range(16, 32))
                def pool_mm(g):
                    for oc in range(2):
                        bks = [main_bank(), main_bank()]
                        fns = []
                        for kc in range(2):
                            for b in range(2):
                                fns.append(lambda e, kc=kc, b=b, oc=oc, g=g, bks=bks: e.matmul(
                                    psb[bks[b]][:, :], wpool[:, g, kc, oc * 128:(oc + 1) * 128],
                                    pooled[:, kc, b * 512:(b + 1) * 512], start=(kc == 0), stop=(kc == 1)))
                        emit_group(PE, fns, reads=[B_parg] + B_pl, writes=[B_ps[bks[0]], B_ps[bks[1]]])
                        for b in range(2):
                            emit(ACT, lambda e, b=b, oc=oc, g=g, bks=bks: e.activation(
                                out=ymix[:, 2 * g + oc, b * 512:(b + 1) * 512], in_=psb[bks[b]][:, :], func=AF.Identity,
                                scale=pscale[:, 2 * g + oc:2 * g + oc + 1]),
                                reads=[B_ps[bks[b]], B_par], writes=[B_ym[2 * g + oc]])
                for g in range(4):
                    w = 2 ** (g + 1)
                    for half in range(2):
                        c = 2 * g + half
                        if half == 0:
                            bs, sv = next_slot(w_in_even_d[g],
                                               [128, KC, 256])
                        bks, bh, fns = zgroup(sv, half, True)
                        emit_group(PE, fns, reads=[bs, B_hh], writes=[B_ps[bks[0]], B_ps[bks[1]], B_ps[bh]])
                        if half == 0 and g >= 1:
                            pool_mm(g - 1)
                        emit(ACT, lambda e, bh=bh: e.activation(out=zb[:, 0:HALO], in_=psb[bh][:, 0:HALO], func=AF.Identity,
                                                                scale=flag),
                             reads=[B_ps[bh], B_par], writes=[B_zb])
                        for b in range(2):
                            emit(ACT, lambda e, b=b, bks=bks: e.activation(
                                out=zb[:, HALO + b * 512:HALO + (b + 1) * 512], in_=psb[bks[b]][:, :], func=AF.Copy),
                                reads=[B_ps[bks[b]]], writes=[B_zb])
                        emit(DVE, lambda e: e.tensor_tensor(out=sA[:, 1:1040], in0=zb[:, 1:1040], in1=zb[:, 0:1039], op=ALU.add),
                             reads=[B_zb], writes=[B_sA])
                        cur, curB = sA, B_sA
                        oth, othB = sB, B_sB
                        k = 2
                        while k < w:
                            emit(DVE, lambda e, k=k, cur=cur, oth=oth: e.tensor_tensor(
                                out=oth[:, 2 * k - 1:1040], in0=cur[:, 2 * k - 1:1040], in1=cur[:, k - 1:1040 - k], op=ALU.add),
                                reads=[curB], writes=[othB])
                            cur, curB, oth, othB = oth, othB, cur, curB
                            k *= 2
                        emit(DVE, lambda e, cur=cur, half=half, w=w: e.scalar_tensor_tensor(
                            out=pooled[:, half, :], in0=cur[:, HALO:1040], scalar=1.0 / w, in1=zb[:, HALO:1040],
                            op0=ALU.mult, op1=ALU.subtract),
                            reads=[curB, B_zb], writes=[B_pl[half]])
                        emit(DVE, lambda e, cur=cur, g=g: e.tensor_tensor(
                            out=t16, in0=cur[:, HALO:2 * HALO], in1=pc[:, 1 + 16 * g:17 + 16 * g], op=ALU.mult),
                            reads=[curB, B_par], writes=[B_t16])
                        emit(DVE, lambda e, half=half: e.tensor_tensor(
                            out=pooled[:, half, 0:HALO], in0=t16, in1=zb[:, HALO:2 * HALO], op=ALU.subtract),
                            reads=[B_t16, B_zb], writes=[B_pl[half]])
                        ada_slots(0, mod_sched.pop(0), 1)
                zv = [arf[:, 0:1040], arf[:, 1040:2080]]
                accs = [arf[:, 2080:3104], rt[:, 0:1024]]
                B_zv = [B_zb, B_sA]
                B_ac = [B_sB, Buf("ac1")]

                def conv_slot(base, pr):
                    return next_slot(w_in_even_d[base // 256 + pr],
                                     [128, KC, 256])

                def some_ada(n):
                    for _ in range(n):
                        if mod_sched:
                            ada_slots(0, mod_sched.pop(0), 1)

                for pr in range(4):
                    sl = conv_slot(1024, pr)
                    for q in range(2):
                        bks, bh, fns = zgroup(sl[1], q, True)
                        emit_group(PE, fns, reads=[sl[0], B_hh], writes=[B_ps[bks[0]], B_ps[bks[1]], B_ps[bh]])
                        if pr == 0 and q == 0:
                            pool_mm(3)
                        emit(ACT, lambda e, bh=bh, q=q: e.activation(out=zv[q][:, 0:HALO], in_=psb[bh][:, 0:HALO], func=AF.Identity,
                                                                     scale=flag),
                             reads=[B_ps[bh], B_par], writes=[B_zv[q]])
                        for b in range(2):
                            emit(ACT, lambda e, b=b, bks=bks, q=q: e.activation(
                                out=zv[q][:, HALO + b * 512:HALO + (b + 1) * 512], in_=psb[bks[b]][:, :], func=AF.Copy),
                                reads=[B_ps[bks[b]]], writes=[B_zv[q]])
                    some_ada(0)
                    sl = conv_slot(3072, pr)
                    for q in range(2):
                        hd = 2 * pr + q
                        bks, bh, fns = zgroup(sl[1], q, True)
                        emit_group(PE, fns, reads=[sl[0], B_hh], writes=[B_ps[bks[0]], B_ps[bks[1]], B_ps[bh]])
                        emit(DVE, lambda e, bh=bh, q=q: e.tensor_tensor(out=zv[q][:, 0:HALO], in0=psb[bh][:, 0:HALO],
                                                                        in1=zv[q][:, 0:HALO], op=ALU.mult),
                             reads=[B_ps[bh], B_zv[q]], writes=[B_zv[q]])
                        for b in range(2):
                            emit(DVE, lambda e, b=b, bks=bks, q=q: e.tensor_tensor(
                                out=zv[q][:, HALO + b * 512:HALO + (b + 1) * 512], in0=psb[bks[b]][:, :],
                                in1=zv[q][:, HALO + b * 512:HALO + (b + 1) * 512], op=ALU.mult),
                                reads=[B_ps[bks[b]], B_zv[q]], writes=[B_zv[q]])
                        emit(ACT, lambda e, hd=hd, q=q: e.activation(out=accs[q], in_=zv[q][:, HALO:HALO + NT], func=AF.Identity,
                                                                     scale=convw[:, 2, hd:hd + 1]),
                             reads=[B_zv[q], B_par], writes=[B_ac[q]])
                        for k in (1, 0):
                            sh = 2 - k
                            emit(DVE, lambda e, hd=hd, k=k, sh=sh, q=q: e.scalar_tensor_tensor(
                                out=accs[q], in0=zv[q][:, HALO - sh:HALO - sh + NT], scalar=convw[:, k, hd:hd + 1],
                                in1=accs[q], op0=ALU.mult, op1=ALU.add),
                                reads=[B_zv[q], B_ac[q], B_par], writes=[B_ac[q]])
                    some_ada(0)
                    sl = conv_slot(2048, pr)
                    for q in range(2):
                        hd = 2 * pr + q
                        bks, bh, fns = zgroup(sl[1], q, False)
                        emit_group(PE, fns, reads=[sl[0]], writes=[B_ps[bks[0]], B_ps[bks[1]]])
                        for b in range(2):
                            emit(DVE, lambda e, b=b, bks=bks, hd=hd, q=q: e.tensor_tensor(
                                out=ymix[:, 8 + hd, b * 512:(b + 1) * 512], in0=psb[bks[b]][:, :],
                                in1=accs[q][:, b * 512:(b + 1) * 512], op=ALU.mult),
                                reads=[B_ps[bks[b]], B_ac[q]], writes=[B_ym[8 + hd]])
                    some_ada(2)
                while mod_sched:
                    ada_slots(0, mod_sched.pop(0), 1)
                w_out_d = w_out_even_d
            else:
                vq = arb[:, 0:16384].rearrange("p (c i f) -> p c i f", c=KC, i=8)
                ug = arb[:, 16384:17408].rearrange("p (b t) -> p b t", b=2)
                B_ug = [Buf("ug0"), Buf("ug1")]
                sgn = arf[:, 0:2048]
                B_sgn = Buf("sgn")
                for t_ in (PE.last, ACT.last, DVE.last):
                    SP.wait(t_)
                emit(SP, lambda e: e.dma_start(out=sgn, in_=sgn_d[0, :].partition_broadcast(128)), writes=[B_sgn], dsem=misc_sem)
                B_vq = [Buf(f"vq{c}") for c in range(KC)]
                sqj = arb[:, 17408:17920].rearrange("p (u q f) -> p u q f", u=2, q=2)
                B_sqj = [Buf("sqj0"), Buf("sqj1")]
                sspart = rt[:, 0:64].rearrange("p (i j) -> p i j", i=8)
                B_ssp = Buf("ssp")
                for j in range(8):
                    bs, sv = next_slot(w_in_odd_d[8 + j],
                                       [128, KC, 256])
                    for i in range(8):
                        bk = main_bank()
                        fns = [(lambda e, kc=kc, i=i, bk=bk, sv=sv: e.matmul(
                            psb[bk][:, 0:256], hT[:, kc, i * 128:(i + 1) * 128], sv[:, kc, :],
                            start=(kc == 0), stop=(kc == KC - 1)), [B_h[kc]]) for kc in range(KC)]
                        emit_group(PE, fns, reads=[bs], writes=[B_ps[bk]])
                        emit(ACT, lambda e, j=j, i=i, bk=bk: e.activation(
                            out=vq[:, 2 * j:2 * j + 2, i, :], in_=psb[bk][:, 0:256].rearrange("p (q f) -> p q f", q=2),
                            func=AF.Gelu_apprx_tanh),
                            reads=[B_ps[bk]], writes=[B_vq[2 * j], B_vq[2 * j + 1]])
                        emit(DVE, lambda e, j=j, i=i: e.scalar_tensor_tensor(
                            out=sqj[:, (i % 2), :, :], in0=vq[:, 2 * j:2 * j + 2, i, :], scalar=1.0, in1=vq[:, 2 * j:2 * j + 2, i, :],
                            op0=ALU.mult, op1=ALU.mult, accum_out=sspart[:, i, j:j + 1]),
                            reads=[B_vq[2 * j], B_vq[2 * j + 1]], writes=[B_sqj[i % 2], B_ssp])
                if stop == "vq0":
                    dump_bf16(arb[:, 0:16384].rearrange("p (c t) -> p c t", c=KC), B_vq)
                    return nc, finish()
                ssv = small[:, 16:24]
                rsv = small[:, 24:32]
                B_ssv = Buf("ssv")
                emit(DVE, lambda e: e.tensor_reduce(out=ssv, in_=sspart, axis=AX.X, op=ALU.add), reads=[B_ssp], writes=[B_ssv])
                emit(ACT, lambda e: e.activation(out=rsv, in_=ssv, func=AF.Sqrt, scale=1.0 / D, bias=epst[:, 0:1]),
                     reads=[B_ssv, B_cst], writes=[B_ssv])
                emit(DVE, lambda e: e.reciprocal(out=rsv, in_=rsv), reads=[B_ssv], writes=[B_ssv])

                def normalize_chunk(c):
                    emit(DVE, lambda e, c=c: e.tensor_tensor(out=vq[:, c, :, :], in0=vq[:, c, :, :],
                                                             in1=rsv.unsqueeze(2).to_broadcast([128, 8, 128]), op=ALU.mult),
                         reads=[B_vq[c], B_ssv], writes=[B_vq[c]])
                    emit(DVE, lambda e, c=c: e.tensor_tensor(out=vq[:, c, :, :], in0=vq[:, c, :, :],
                                                             in1=sgn[:, c * 128:(c + 1) * 128].unsqueeze(1).to_broadcast([128, 8, 128]),
                                                             op=ALU.mult),
                         reads=[B_vq[c], B_sgn], writes=[B_vq[c]])

                if stop == "vq1":
                    for c in range(KC):
                        normalize_chunk(c)
                    dump_bf16(arb[:, 0:16384].rearrange("p (c t) -> p c t", c=KC), B_vq)
                    return nc, finish()
                for j in range(8):
                    bs, sv = next_slot(w_in_odd_d[j],
                                       [128, KC, 256])
                    for q in range(2):
                        c = 2 * j + q
                        hd = c // 2
                        normalize_chunk(c)
                        bks = [main_bank(), main_bank()]
                        fns = []
                        for kc in range(KC):
                            for b in range(2):
                                fns.append(lambda e, kc=kc, b=b, q=q, sv=sv, bks=bks: e.matmul(
                                    psb[bks[b]][:, :], sv[:, kc, q * 128:(q + 1) * 128], hT[:, kc, b * 512:(b + 1) * 512],
                                    start=(kc == 0), stop=(kc == KC - 1)))
                        emit_group(PE, fns, reads=[bs] + B_h, writes=[B_ps[bks[0]], B_ps[bks[1]]])
                        for b in range(2):
                            emit(ACT, lambda e, b=b, bks=bks: e.activation(
                                out=ug[:, b, :], in_=psb[bks[b]][:, :], func=AF.Gelu_apprx_tanh),
                                reads=[B_ps[bks[b]]], writes=[B_ug[b]])
                        sbk = [main_bank(), main_bank()]
                        fns = []
                        for i in range(8):
                            o = psb[sbk[i // 4]][:, (i % 4) * 128:(i % 4 + 1) * 128]
                            fns.append(lambda e, o=o, hd=hd: e.matmul(o, ones_bf[:], bmat[:, hd, :], start=True, stop=False))
                            fns.append(lambda e, o=o, hd=hd, c=c, i=i: e.matmul(o, vq[:, c, i, :], wspT[:, hd, :],
                                                                                start=False, stop=True))
                        emit_group(PE, fns, reads=[B_vq[c], B_wsp, B_parg, B_cst], writes=[B_ps[sbk[0]], B_ps[sbk[1]]])
                        for b in range(2):
                            emit(DVE, lambda e, b=b, c=c, sbk=sbk: e.tensor_tensor(
                                out=ymix[:, c, b * 512:(b + 1) * 512], in0=psb[sbk[b]][:, :], in1=ug[:, b, :], op=ALU.mult),
                                reads=[B_ps[sbk[b]], B_ug[b]], writes=[B_vq[c]])
                B_ym = B_vq
                w_out_d = w_out_odd_d
            if stop == f"ymix{l}":
                dump_bf16(ymix, B_ym)
                return nc, finish()

            mix_end = [PE.last, ACT.last, DVE.last]
            for j in range(8):
                bs, sv = next_slot(w_out_d[j], [128, KC, 256])
                for q in range(2):
                    dc = 2 * j + q
                    bks = [main_bank(), main_bank()]
                    fns = []
                    for kc in range(KC):
                        for b in range(2):
                            fns.append(lambda e, kc=kc, b=b, q=q, sv=sv, bks=bks: e.matmul(
                                psb[bks[b]][:, :], sv[:, kc, q * 128:(q + 1) * 128], ymix[:, kc, b * 512:(b + 1) * 512],
                                start=(kc == 0), stop=(kc == KC - 1)))
                    emit_group(PE, fns, reads=[bs] + B_ym, writes=[B_ps[bks[0]], B_ps[bks[1]]])
                    for b in range(2):
                        emit(DVE, lambda e, b=b, dc=dc, bks=bks, l=l: e.scalar_tensor_tensor(
                            out=xT[:, dc, b * 512:(b + 1) * 512], in0=psb[bks[b]][:, :], scalar=mod(l, 2)[:, dc:dc + 1],
                            in1=xT[:, dc, b * 512:(b + 1) * 512], op0=ALU.mult, op1=ALU.add),
                            reads=[B_ps[bks[b]], B_mod[l], B_x[dc][b]], writes=[B_x[dc][b]])
                if l == 0:
                    ada_slots(0, 32 + j, 1)
            if stop == f"mix{l}":
                dump_f32(xT)
                return nc, finish()

            emit(DVE, lambda e, l=l: e.scalar_tensor_tensor(out=a2[:, l, :], in0=mod(l, 4), scalar=1.0, in1=nffn[:, l, :],
                                                            op0=ALU.add, op1=ALU.mult),
                 reads=[B_mod[l], B_par], writes=[B_mod[l]])
            for e_ in (ACT, DVE):
                for t_ in mix_end:
                    e_.wait(t_)
            B_rstd = rms_stats(f"n2_{l}")
            B_tmp = [Buf(f"tmpb{i}") for i in range(2)]
            B_hf = [Buf(f"hf{i}") for i in range(2)]
            lbk = [aux_bank(), aux_bank()]
            for c in range(KC):
                for b in range(2):
                    i = b
                    emit(DVE, lambda e, c=c, b=b, i=i, l=l: e.scalar_tensor_tensor(
                        out=tmpv[:, i, :], in0=xT[:, c, b * 512:(b + 1) * 512], scalar=a2[:, l, c:c + 1],
                        in1=rstd[:, b * 512:(b + 1) * 512], op0=ALU.mult, op1=ALU.mult),
                        reads=[B_x[c][b], B_rstd, B_mod[l]], writes=[B_tmp[i]])
                    emit(ACT, lambda e, c=c, b=b, i=i, l=l: e.activation(
                        out=hfv[:, i, :], in_=tmpv[:, i, :], func=AF.Identity, bias=mod(l, 3)[:, c:c + 1], scale=1.0),
                        reads=[B_tmp[i], B_mod[l]], writes=[B_hf[i]])
                    emit(DVE, lambda e, c=c, b=b, i=i: e.tensor_copy(out=hT[:, c, b * 512:(b + 1) * 512], in_=hfv[:, i, :]),
                         reads=[B_hf[i]], writes=[B_h[c]])
                    emit_group(PE, [lambda e, c=c, b=b, i=i: e.matmul(
                        psb[lbk[b]][0:16, :], wr[:, c, :], hfv[:, i, :], start=(c == 0), stop=(c == KC - 1))],
                        reads=[B_hf[i], B_par], writes=[B_ps[lbk[b]]])
            if stop == f"h2_{l}":
                dump_bf16(hT, B_h)
                return nc, finish()
            B_lg = Buf("lg")
            for b in range(2):
                emit(ACT, lambda e, b=b: e.activation(out=lgT[:, b * 512:(b + 1) * 512], in_=psb[lbk[b]][0:16, :], func=AF.Sigmoid),
                     reads=[B_ps[lbk[b]]], writes=[B_lg])
            bk = aux_bank()
            fns = [lambda e, i=i, bk=bk: e.transpose(psb[bk][:, i * 16:(i + 1) * 16], lgT[:, i * 128:(i + 1) * 128], ident[0:16, 0:16])
                   for i in range(8)]
            emit_group(PE, fns, reads=[B_lg, B_par], writes=[B_ps[bk]])
            aff = rt[:, 0:128]
            sel = rt[:, 128:256]
            eq1 = rt[:, 256:384]
            sel2 = rt[:, 384:512]
            ge2 = rt[:, 512:640]
            wts = rt[:, 640:768]
            comb = rt[:, 768:896]
            m1 = rt[:, 896:928]
            m2 = rt[:, 928:960]
            gs = rt[:, 960:992]
            gmax = rt[:, 992:1000]
            gmask = rt[:, 1000:1032]
            den = rt[:, 1032:1040]
            B_rt = Buf("rt")

            def g4(ap):
                return ap.rearrange("p (a k) -> p a k", k=4)

            def bl(ap, n):
                return ap.unsqueeze(2).to_broadcast([128, ap.shape[1], n])

            R = dict(reads=[B_rt], writes=[B_rt])
            emit(DVE, lambda e, bk=bk: e.tensor_copy(out=aff, in_=psb[bk][:, 0:128]), reads=[B_ps[bk]], writes=[B_rt])
            emit(DVE, lambda e: e.tensor_tensor(out=sel.rearrange("p (i x) -> p i x", i=8), in0=aff.rearrange("p (i x) -> p i x", i=8),
                                                in1=rbias[:].unsqueeze(1).to_broadcast([128, 8, NE]), op=ALU.add),
                 reads=[B_rt, B_par], writes=[B_rt])
            emit(DVE, lambda e: e.tensor_reduce(out=m1, in_=g4(sel), axis=AX.X, op=ALU.max), **R)
            emit(DVE, lambda e: e.tensor_tensor(out=g4(eq1), in0=g4(sel), in1=bl(m1, 4), op=ALU.is_equal), **R)
            emit(DVE, lambda e: e.scalar_tensor_tensor(out=sel2, in0=eq1, scalar=-1e30, in1=sel, op0=ALU.mult, op1=ALU.add), **R)
            emit(DVE, lambda e: e.tensor_reduce(out=m2, in_=g4(sel2), axis=AX.X, op=ALU.max), **R)
            emit(DVE, lambda e: e.tensor_tensor(out=gs, in0=m1, in1=m2, op=ALU.add), **R)
            emit(DVE, lambda e: e.tensor_reduce(out=gmax, in_=g4(gs), axis=AX.X, op=ALU.max), **R)
            emit(DVE, lambda e: e.tensor_tensor(out=g4(gmask), in0=g4(gs), in1=bl(gmax, 4), op=ALU.is_equal), **R)
            emit(DVE, lambda e: e.tensor_tensor(out=g4(ge2), in0=g4(sel), in1=bl(m2, 4), op=ALU.is_ge), **R)
            emit(DVE, lambda e: e.tensor_tensor(out=g4(ge2), in0=g4(ge2), in1=bl(gmask, 4), op=ALU.mult), **R)
            emit(DVE, lambda e: e.tensor_tensor(out=wts, in0=aff, in1=ge2, op=ALU.mult), **R)
            emit(DVE, lambda e: e.tensor_reduce(out=den, in_=wts.rearrange("p (i x) -> p i x", i=8), axis=AX.X, op=ALU.add), **R)
            emit(DVE, lambda e: e.reciprocal(out=den, in_=den), **R)
            emit(DVE, lambda e: e.tensor_tensor(out=comb.rearrange("p (i x) -> p i x", i=8), in0=wts.rearrange("p (i x) -> p i x", i=8),
                                                in1=bl(den, NE), op=ALU.mult), **R)
            hib = arb[:, 0:128].rearrange("p (i x) -> p i x", i=8)
            chl = rt[:, 256:512].rearrange("p (i x) -> p i x", i=8)
            comb3 = comb.rearrange("p (i x) -> p i x", i=8)
            emit(DVE, lambda e: e.tensor_copy(out=hib, in_=comb3), **R)
            emit(DVE, lambda e: e.tensor_copy(out=chl[:, :, 0:16], in_=hib), **R)
            emit(DVE, lambda e: e.tensor_tensor(out=chl[:, :, 16:32], in0=comb3, in1=chl[:, :, 0:16], op=ALU.subtract), **R)
            bkc = [aux_bank(), aux_bank()]
            fns = [lambda e, i=i: e.transpose(psb[bkc[i // 4]][0:32, (i % 4) * 128:(i % 4 + 1) * 128], chl[:, i, :], ident[:])
                   for i in range(8)]
            emit_group(PE, fns, reads=[B_rt, B_par], writes=[B_ps[bkc[0]], B_ps[bkc[1]]])
            for b in range(2):
                emit(ACT, lambda e, b=b: e.activation(out=combT[:, b * 512:(b + 1) * 512], in_=psb[bkc[b]][0:32, :], func=AF.Copy),
                     reads=[B_ps[bkc[b]]], writes=[B_combT])
            if stop == f"comb{l}":
                barrier()
                emit(DVE, lambda e: e.memset(xT[:, 0, :], 0.0), writes=[B_x[0][0], B_x[0][1]])
                emit(DVE, lambda e: e.tensor_copy(out=xT[0:32, 0, :], in_=combT[:]), reads=[B_combT], writes=[B_x[0][0], B_x[0][1]])
                dump_f32(xT)
                return nc, finish()
            barrier()

            aT = arb[:, 0:8192].rearrange("p (u f t) -> p u f t", u=2, f=4)
            sg = arf[:, 2048:3072].rearrange("p (u t) -> p u t", u=2)
            cb = arf[:, 0:2048].rearrange("p (u t) -> p u t", u=2)
            B_aT = [[Buf(f"aT{u}_{f}") for f in range(4)] for u in range(2)]
            B_sg = [Buf("sg0"), Buf("sg1")]
            B_cb = [Buf("cb0"), Buf("cb1")]
            B_sel = [Buf("sel0"), Buf("sel1")]
            nxt_ada = ([(0, j) for j in range(40, 48)] + [(1, j) for j in range(48)]) if l == 0 else []
            set_ring(RING + 2, gate_toks=[PE.last, ACT.last, DVE.last])
            sgi = [0]

            def gate_up(ex):
                u = ex % 2
                bks = [aux_bank(), aux_bank()]
                emit(DVE, lambda e, ex=ex, u=u: e.tensor_tensor(out=selmat[:, u, :], in0=ident[0:32, ex:ex + 1].to_broadcast([32, 128]),
                                                                in1=ident[0:32, 16 + ex:17 + ex].to_broadcast([32, 128]), op=ALU.add),
                     reads=[B_par], writes=[B_sel[u]])
                fns = [lambda e, b=b, u=u, bks=bks: e.matmul(psb[bks[b]][:, :], selmat[:, u, :], combT[:, b * 512:(b + 1) * 512],
                                                             start=True, stop=True) for b in range(2)]
                emit_group(PE, fns, reads=[B_combT, B_sel[u]], writes=[B_ps[bks[0]], B_ps[bks[1]]])
                for b in range(2):
                    emit(ACT, lambda e, b=b, u=u, bks=bks: e.activation(out=cb[:, u, b * 512:(b + 1) * 512], in_=psb[bks[b]][:, :],
                                                                        func=AF.Copy),
                         reads=[B_ps[bks[b]]], writes=[B_cb[u]])
                for half in range(2):
                    gs_ = next_slot(wg_d[l, ex, half], [128, KC, 256])
                    us_ = next_slot(wu_d[l, ex, half], [128, KC, 256])
                    for q in range(2):
                        fc = 2 * half + q
                        for b in range(2):
                            bg, bu = main_bank(), main_bank()
                            fns = []
                            for kc in range(KC):
                                fns.append((lambda e, kc=kc, q=q, b=b, bg=bg, sv=gs_[1]: e.matmul(
                                    psb[bg][:, :], sv[:, kc, q * 128:(q + 1) * 128], hT[:, kc, b * 512:(b + 1) * 512],
                                    start=(kc == 0), stop=(kc == KC - 1)), [B_h[kc]]))
                            for kc in range(KC):
                                fns.append(lambda e, kc=kc, q=q, b=b, bu=bu, sv=us_[1]: e.matmul(
                                    psb[bu][:, :], sv[:, kc, q * 128:(q + 1) * 128], hT[:, kc, b * 512:(b + 1) * 512],
                                    start=(kc == 0), stop=(kc == KC - 1)))
                            emit_group(PE, fns, reads=[gs_[0], us_[0]], writes=[B_ps[bg], B_ps[bu]])
                            si = sgi[0]
                            sgi[0] = 1 - si
                            emit(ACT, lambda e, bg=bg, si=si: e.activation(out=sg[:, si, :], in_=psb[bg][:, :], func=AF.Silu),
                                 reads=[B_ps[bg]], writes=[B_sg[si]])
                            emit(DVE, lambda e, si=si, u=u, b=b: e.tensor_tensor(
                                out=sg[:, si, :], in0=sg[:, si, :], in1=cb[:, u, b * 512:(b + 1) * 512], op=ALU.mult),
                                reads=[B_sg[si], B_cb[u]], writes=[B_sg[si]])
                            emit(DVE, lambda e, si=si, u=u, b=b, fc=fc, bu=bu: e.tensor_tensor(
                                out=aT[:, u, fc, b * 512:(b + 1) * 512], in0=psb[bu][:, :], in1=sg[:, si, :], op=ALU.mult),
                                reads=[B_ps[bu], B_sg[si]], writes=[B_aT[u][fc]])

            def down(ex):
                u = ex % 2
                for half in range(2):
                    ds_ = next_slot(wd_d[l, ex, half], [128, 4, 1024])
                    for q in range(8):
                        dc = 8 * half + q
                        for b in range(2):
                            bk = main_bank()
                            fns = [lambda e, kc=kc, q=q, b=b, bk=bk, sv=ds_[1]: e.matmul(
                                psb[bk][:, :], sv[:, kc, q * 128:(q + 1) * 128], aT[:, u, kc, b * 512:(b + 1) * 512],
                                start=(kc == 0), stop=(kc == 3)) for kc in range(4)]
                            emit_group(PE, fns, reads=[ds_[0]] + B_aT[u], writes=[B_ps[bk]])
                            emit(DVE, lambda e, b=b, dc=dc, bk=bk, l=l: e.scalar_tensor_tensor(
                                out=xT[:, dc, b * 512:(b + 1) * 512], in0=psb[bk][:, :], scalar=mod(l, 5)[:, dc:dc + 1],
                                in1=xT[:, dc, b * 512:(b + 1) * 512], op0=ALU.mult, op1=ALU.add),
                                reads=[B_ps[bk], B_mod[l], B_x[dc][b]], writes=[B_x[dc][b]])

            nex = NE
            for ex in range(nex):
                gate_up(ex)
                if l == 0 and ex <= 1:
                    for _ in range(4):
                        l_, j_ = nxt_ada.pop(0)
                        ada_slots(l_, j_, 1)
                if ex >= 1:
                    down(ex - 1)
                if ex >= 2:
                    for _ in range(4):
                        if nxt_ada:
                            l_, j_ = nxt_ada.pop(0)
                            ada_slots(l_, j_, 1)
            down(nex - 1)
            while nxt_ada:
                l_, j_ = nxt_ada.pop(0)
                ada_slots(l_, j_, 1)
            set_ring(RING)
            if stop == f"moe{l}":
                dump_f32(xT)
                return nc, finish()

        rstd_fin = rt[:, 0:1024]
        B_rstd = rms_stats("fin", rd=rstd_fin)
        for c in range(KC):
            for b in range(2):
                emit(DVE, lambda e, c=c, b=b: e.scalar_tensor_tensor(
                    out=xT[:, c, b * 512:(b + 1) * 512], in0=xT[:, c, b * 512:(b + 1) * 512], scalar=fnorm[:, c:c + 1],
                    in1=rstd_fin[:, b * 512:(b + 1) * 512], op0=ALU.mult, op1=ALU.mult),
                    reads=[B_x[c][b], B_rstd, B_par], writes=[B_x[c][b]])
            emit(SP, lambda e, c=c: e.dma_start(out=outT_d[:, c, :], in_=xT[:, c, :]),
                 reads=[B_x[c][0], B_x[c][1]], dsem=out_sem)
        return nc, finish()


def _fm(v):
    v = np.asarray(v, np.float32)
    lead = v.shape[:-1]
    n = v.shape[-1] // 128
    return np.ascontiguousarray(np.moveaxis(v.reshape(*lead, n, 128), -1, 0))


def _slots(w, ncol):
    w = np.asarray(w, np.float32)
    lead = w.shape[:-2]
    K, N = w.shape[-2:]
    r = w.reshape(*lead, K // 128, 128, N // ncol, ncol)
    nl = len(lead)
    r = np.transpose(r, tuple(range(nl)) + (nl + 2, nl + 1, nl + 0, nl + 3))
    return np.ascontiguousarray(r).reshape(*lead, N // ncol, 128, (K // 128) * ncol)


def make_in_maps(inp):
    f = lambda a: np.ascontiguousarray(np.asarray(a, np.float32))
    x = f(inp["x"])
    shared = {
        "ada_w": _slots(inp["ada_w"], 256),
        "ada_bT": _fm(inp["ada_b"]),
        "nmixT": _fm(inp["norm_mix"]),
        "nffnT": _fm(inp["norm_ffn"]),
        "fnormT": _fm(inp["final_norm"]),
        "w_in_even": _slots(inp["w_in_even"][0], 256),
        "w_pool": f(inp["w_pool"][0]),
        "pscaleT": _fm(inp["pool_scale"][0]),
        "convwT": np.ascontiguousarray(np.transpose(f(inp["conv_w"][0]), (2, 0, 1))),
        "w_out_even": _slots(inp["w_out_even"][0], 256),
        "w_in_odd": _slots(inp["w_in_odd"][0], 256),
        "sgu_norm": f(inp["sgu_norm"]).reshape(1, D),
        "w_spatial": f(inp["w_spatial"][0]),
        "b_spatial": f(inp["b_spatial"][0]).reshape(1, 8 * 128),
        "w_out_odd": _slots(inp["w_out_odd"][0], 256),
        "w_router": f(inp["w_router"]),
        "router_bias": f(inp["router_bias"]).reshape(1, NE),
        "w_gate": _slots(inp["w_gate"], 256),
        "w_up": _slots(inp["w_up"], 256),
        "w_down": _slots(inp["w_down"], 1024),
        "ident": np.eye(128, dtype=np.float32),
        "trilT": np.triu(np.ones((128, 128), np.float32)),
    }
    maps = []
    for core in range(NCORES):
        b, half = core // 2, core % 2
        t0 = half * NT
        xs = x[b, t0:t0 + NT]
        xT = np.ascontiguousarray(np.transpose(xs.reshape(NT, KC, 128), (2, 1, 0)))
        if half == 1:
            xh = x[b, t0 - HALO:t0]
        else:
            xh = np.zeros((HALO, D), np.float32)
        xhT = np.ascontiguousarray(np.transpose(xh.reshape(HALO, KC, 128), (2, 1, 0)))
        pcv = np.zeros((128, 65), np.float32)
        pcv[:, 0] = float(half)
        for g, w in enumerate((2, 4, 8, 16)):
            pos = np.arange(t0 + 1, t0 + 17, dtype=np.float32)
            pcv[:, 1 + 16 * g:17 + 16 * g] = (1.0 / np.minimum(pos, float(w)))[None, :]
        m = dict(shared)
        m["xT"] = xT
        m["xhT"] = xhT
        m["cT"] = _fm(inp["c"][b])
        m["pc"] = pcv
        maps.append(m)
    return maps


_NC_CACHE = {}


def kernel(**inputs):
    if "nc" not in _NC_CACHE:
        _NC_CACHE["nc"] = build()[0]
    nc = _NC_CACHE["nc"]
    maps = make_in_maps(inputs)
    res = run_bass_kernel_spmd(nc, maps, core_ids=list(range(NCORES)))
    out = np.empty((4, 2 * NT, D), np.float32)
    for core in range(NCORES):
        b, half = core // 2, core % 2
        oT = np.asarray(res.results[core]["outT"])
        out[b, half * NT:(half + 1) * NT] = np.transpose(oT, (2, 1, 0)).reshape(NT, D)
    return out
```

```python
import numpy as np
from contextlib import ExitStack
import concourse.bass as bass
import concourse.mybir as mybir
from concourse.bass_utils import run_bass_kernel_spmd

F32 = mybir.dt.float32
BF16 = mybir.dt.bfloat16
AF = mybir.ActivationFunctionType
ALU = mybir.AluOpType
AX = mybir.AxisListType

D = 2048
NT = 1024
KC = 16
HALO = 16
NE = 16
EPS = 1e-6
RING = 5
SLOT = 4096
NCORES = 8


class Eng:
    def __init__(self, name, nc, stack, nsem, limit=4000):
        self.name = name
        self.ops = []
        self.sems = [stack.enter_context(nc.semaphore(f"s_{name}_{i}")) for i in range(nsem)]
        self.cur = 0
        self.count = 0
        self.limit = limit
        self.waited = {}
        self.last = None

    def new_token(self):
        if self.count >= self.limit:
            self.cur += 1
            self.count = 0
        self.count += 1
        tok = (self.sems[self.cur], self.count, (self.name, self.cur), 1)
        self.last = tok
        return tok

    def wait(self, tok):
        if tok is None:
            return
        key = tok[2]
        if self.waited.get(key, 0) >= tok[1]:
            return
        self.waited[key] = tok[1]
        self.ops.append(("w", tok[0], tok[1]))


class DmaSem:
    def __init__(self, name, nc, stack):
        self.name = name
        self.sem = stack.enter_context(nc.semaphore(f"d_{name}"))
        self.count = 0

    def new_token(self):
        self.count += 1
        return (self.sem, 16 * self.count, ("dma", self.name), 16)


class Buf:
    def __init__(self, name, const=False):
        self.name = name
        self.w = None
        self.r = {}
        self.const = const


def _deps(E, reads, writes):
    for b in reads:
        if b.w is not None and not (E.name == "pe" and b.w[2][0] == "pe"):
            E.wait(b.w)
    for b in writes:
        if b.w is not None and not (E.name == "pe" and b.w[2][0] == "pe"):
            E.wait(b.w)
        for t in b.r.values():
            if not (E.name == "pe" and t[2][0] == "pe"):
                E.wait(t)


def _commit(tok, reads, writes, rkey):
    for b in reads:
        if not b.const:
            b.r[rkey] = tok
    for b in writes:
        b.w = tok
        b.r = {}


def emit(E, fn, reads=(), writes=(), dsem=None):
    _deps(E, reads, writes)
    if dsem is not None:
        tok = dsem.new_token()
        rkey = ("dma", dsem.name, dsem.count)
    else:
        tok = E.new_token()
        rkey = E.name
    E.ops.append(("i", fn, tok))
    _commit(tok, reads, writes, rkey)
    return tok


def emit_group(E, fns, reads=(), writes=()):
    _deps(E, reads, writes)
    tok = E.new_token()
    allreads = list(reads)
    for i, f in enumerate(fns):
        if isinstance(f, tuple):
            fn, rds = f
            _deps(E, rds, ())
            for r in rds:
                if r not in allreads:
                    allreads.append(r)
        else:
            fn = f
        E.ops.append(("i", fn, tok if i == len(fns) - 1 else None))
    _commit(tok, allreads, writes, E.name)
    return tok


def replay(E, h):
    for op in E.ops:
        if op[0] == "w":
            h.wait_ge(op[1], op[2])
        else:
            inst = op[1](h)
            if op[2] is not None:
                inst.then_inc(op[2][0], op[2][3])


def build(stop=None):
    nc = bass.Bass("TRN2", target_bir_lowering=False)

    def din(name, shape):
        return nc.dram_tensor(name, list(shape), F32, kind="ExternalInput").ap()

    xT_d = din("xT", [128, KC, NT])
    xhT_d = din("xhT", [128, KC, HALO])
    cT_d = din("cT", [128, KC])
    pc_d = din("pc", [128, 65])
    ada_w_d = din("ada_w", [2, D, 6 * D])
    ada_bT_d = din("ada_bT", [128, 2, 96])
    nmix_d = din("nmixT", [128, 2, KC])
    nffn_d = din("nffnT", [128, 2, KC])
    fnorm_d = din("fnormT", [128, KC])
    w_in_even_d = din("w_in_even", [D, 2 * D])
    w_pool_d = din("w_pool", [4, 256, 256])
    pscale_d = din("pscaleT", [128, 8])
    convw_d = din("convwT", [128, 3, 8])
    w_out_even_d = din("w_out_even", [D, D])
    w_in_odd_d = din("w_in_odd", [D, 2 * D])
    sgn_d = din("sgu_norm", [1, D])
    wsp_d = din("w_spatial", [8, 128, 128])
    bsp_d = din("b_spatial", [1, 8 * 128])
    w_out_odd_d = din("w_out_odd", [D, D])
    wr_d = din("w_router", [D, NE])
    rb_d = din("router_bias", [1, NE])
    wg_d = din("w_gate", [2, NE, D, 512])
    wu_d = din("w_up", [2, NE, D, 512])
    wd_d = din("w_down", [2, NE, 512, D])
    ident_d = din("ident", [128, 128])
    tril_d = din("trilT", [128, 128])
    outT_d = nc.dram_tensor("outT", [128, KC, NT], F32, kind="ExternalOutput").ap()

    with ExitStack() as st:
        def sb(name, shape, dt):
            return st.enter_context(nc.sbuf_tensor("sb_" + name, list(shape), dt))

        xT = sb("xT", [128, KC, NT], F32)
        hT = sb("hT", [128, KC, NT], BF16)
        xh = sb("xh", [128, KC, HALO], F32)
        hh = sb("hh", [128, KC, HALO], BF16)
        ring = sb("ring", [128, RING, SLOT], BF16)
        arb = sb("arb", [128, 18432], BF16)
        arf = sb("arf", [128, 3136], F32)
        wspn = arf[:, 0:1024].rearrange("p (h s) -> p h s", h=8)
        cT = sb("cT", [128, KC], F32)
        cact = sb("cact", [128, KC], BF16)
        pc = sb("pc", [128, 65], F32)
        ada_bT = sb("ada_bT", [128, 2, 96], F32)
        modsb = sb("modsb", [128, 2, 96], F32)
        nmix = sb("nmix", [128, 2, KC], F32)
        nffn = sb("nffn", [128, 2, KC], F32)
        fnorm = sb("fnorm", [128, KC], F32)
        a1 = sb("a1", [128, 2, KC], F32)
        a2 = sb("a2", [128, 2, KC], F32)
        pscale = sb("pscale", [128, 8], F32)
        convw = sb("convw", [128, 3, 8], F32)
        wpool = sb("wpool", [128, 4, 2, 256], BF16)
        wspT = sb("wspT", [128, 8, 128], BF16)
        bmat = sb("bmat", [128, 8, 128], BF16)
        wr = sb("wr", [128, KC, NE], F32)
        rbias = sb("rbias", [128, NE], F32)
        ident = sb("ident", [128, 128], F32)
        tril = sb("tril", [128, 128], F32)
        ones_bf = sb("ones_bf", [128, 128], BF16)
        selmat = sb("selmat", [32, 2, 128], BF16)
        combT = sb("combThl", [32, 1024], BF16)
        epst = sb("epst", [128, 1], F32)
        rt = sb("rt", [128, 1040], F32)
        small = sb("small", [128, 64], F32)

        psb = [st.enter_context(nc.psum_tensor(f"ps{i}", [128, 512], F32)) for i in range(8)]

        PE = Eng("pe", nc, st, 6)
        ACT = Eng("act", nc, st, 4)
        DVE = Eng("dve", nc, st, 6)
        POOL = Eng("pool", nc, st, 2)
        SP = Eng("sp", nc, st, 2)
        slot_sems = [DmaSem(f"slot{i}", nc, st) for i in range(RING + 2)]
        par_sem = DmaSem("par", nc, st)
        parg_sem = DmaSem("parg", nc, st)
        x_sem = DmaSem("x", nc, st)
        misc_sem = DmaSem("misc", nc, st)
        out_sem = DmaSem("out", nc, st)

        block = st.enter_context(nc.Block())

        B_x = [[Buf(f"x{c}_{b}") for b in range(2)] for c in range(KC)]
        B_h = [Buf(f"h{c}") for c in range(KC)]
        B_hh = Buf("hh")
        B_slot = [Buf(f"slot{i}") for i in range(RING + 2)]
        B_ps = [Buf(f"ps{i}") for i in range(8)]
        B_par = Buf("par", const=True)
        B_mod = [Buf(f"mod{l}") for l in range(2)]
        state = {"main": 0, "aux": 0, "pos": 0, "ring": list(range(RING)), "gate": {}}

        def main_bank():
            i = state["main"]
            state["main"] = (i + 1) % 6
            return i

        def aux_bank():
            i = 6 + state["aux"]
            state["aux"] = (state["aux"] + 1) % 2
            return i

        def barrier():
            lasts = [PE.last, ACT.last, DVE.last]
            for e in (ACT, DVE):
                for t in lasts:
                    if t is not None:
                        e.wait(t)

        def set_ring(n, gate_toks=None):
            state["ring"] = list(range(n))
            state["pos"] = 0
            state["gate"] = {i: gate_toks for i in range(RING, n)} if gate_toks else {}

        def next_slot(src_ap, view):
            rl = state["ring"]
            i = rl[state["pos"] % len(rl)]
            state["pos"] += 1
            nelem = 1
            for s_ in view[1:]:
                nelem *= s_
            if i < RING:
                dst = ring[:, i, 0:nelem]
            else:
                o = 8192 + (i - RING) * SLOT
                dst = arb[:, o:o + nelem]
                if i in state["gate"]:
                    for t_ in state["gate"].pop(i):
                        POOL.wait(t_)
            if len(view) == 3:
                dst = dst.rearrange("p (a b) -> p a b", a=view[1])
            emit(POOL, lambda e, dst=dst, src=src_ap: e.dma_start(out=dst, in_=src),
                 reads=[], writes=[B_slot[i]], dsem=slot_sems[i])
            return B_slot[i], dst

        def spload(dst, src, sem=par_sem):
            emit(SP, lambda e, dst=dst, src=src: e.dma_start(out=dst, in_=src), dsem=sem)

        for c in range(KC):
            emit(SP, lambda e, c=c: e.dma_start(out=xT[:, c, :], in_=xT_d[:, c, :]),
                 writes=[B_x[c][0], B_x[c][1]], dsem=x_sem)
        xtok = (x_sem.sem, 16 * x_sem.count, ("dma", "x"), 16)
        for c in range(KC):
            for b in range(2):
                B_x[c][b].w = xtok
        spload(xh[:], xhT_d)
        spload(cT[:], cT_d)
        spload(pc[:], pc_d)
        spload(ada_bT[:], ada_bT_d)
        spload(nmix[:], nmix_d)
        spload(nffn[:], nffn_d)
        spload(fnorm[:], fnorm_d)
        spload(pscale[:], pscale_d)
        spload(convw[:], convw_d)
        spload(wspn, wsp_d.rearrange("h t s -> t h s"))
        spload(wr[:], wr_d.rearrange("(kc p) e -> p kc e", p=128))
        spload(rbias[:], rb_d[0, :].partition_broadcast(128))
        spload(ident[:], ident_d)
        spload(tril[:], tril_d)
        B_par.w = (par_sem.sem, 16 * par_sem.count, ("dma", "par"), 16)

        B_cst = Buf("cst")
        emit(DVE, lambda e: e.memset(ones_bf[:], 1.0), writes=[B_cst])
        emit(DVE, lambda e: e.memset(epst[:], EPS), writes=[B_cst])
        emit(DVE, lambda e: e.memset(bmat[:], 0.0), writes=[B_cst])
        emit(POOL, lambda e: e.dma_start(out=wpool[:], in_=w_pool_d.rearrange("g (kc p) n -> p g kc n", p=128)),
             dsem=parg_sem)
        emit(POOL, lambda e: e.dma_start(out=bmat[0:1, :, :], in_=bsp_d.rearrange("o (h t) -> o h t", h=8)),
             reads=[B_cst], dsem=parg_sem)
        B_parg = Buf("parg", const=True)
        B_parg.w = (parg_sem.sem, 16 * parg_sem.count, ("dma", "parg"), 16)
        B_cst.const = True

        emit(ACT, lambda e: e.activation(out=cact[:], in_=cT[:], func=AF.Silu), reads=[B_par], writes=[B_cst])

        B_wsp = Buf("wsp")
        for half in range(2):
            bk = aux_bank()
            fns = []
            for q in range(4):
                hd = half * 4 + q
                fns.append(lambda e, hd=hd, q=q, bk=bk: e.transpose(psb[bk][:, q * 128:(q + 1) * 128], wspn[:, hd, :], ident[:]))
            emit_group(PE, fns, reads=[B_par], writes=[B_ps[bk]])
            emit(DVE, lambda e, half=half, bk=bk: e.tensor_tensor(
                out=wspT[:, half * 4:(half + 1) * 4, :],
                in0=psb[bk][:].rearrange("p (q t) -> p q t", q=4),
                in1=tril[:].unsqueeze(1).to_broadcast([128, 4, 128]), op=ALU.mult),
                reads=[B_ps[bk], B_par], writes=[B_wsp])

        def ada_slots(l, j0, nslots):
            for s in range(j0, j0 + nslots):
                src = ada_w_d[l][:, 256 * s:256 * (s + 1)].rearrange("(kc p) n -> p kc n", p=128)
                bs, sv = next_slot(src, [128, KC, 256])
                bk = aux_bank()
                fns = []
                for q in range(2):
                    for kc in range(KC):
                        fns.append(lambda e, q=q, kc=kc, sv=sv, bk=bk: e.matmul(
                            psb[bk][:, q:q + 1], sv[:, kc, q * 128:(q + 1) * 128], cact[:, kc:kc + 1],
                            start=(kc == 0), stop=(kc == KC - 1)))
                emit_group(PE, fns, reads=[bs, B_cst], writes=[B_ps[bk]])
                emit(DVE, lambda e, s=s, bk=bk, l=l: e.tensor_tensor(
                    out=modsb[:, l, 2 * s:2 * s + 2], in0=psb[bk][:, 0:2], in1=ada_bT[:, l, 2 * s:2 * s + 2], op=ALU.add),
                    reads=[B_ps[bk], B_par], writes=[B_mod[l]])

        def mod(l, k):
            return modsb[:, l, 16 * k:16 * (k + 1)]

        sqv = arb[:, 16384:18432].rearrange("p (b t) -> p b t", b=2)
        rstd = arf[:, 0:1024]
        tmpv = arf[:, 1024:2048].rearrange("p (b t) -> p b t", b=2)
        hfv = arf[:, 2048:3072].rearrange("p (b t) -> p b t", b=2)
        lgT = rt[0:16, 0:1024]
        B_combT = Buf("combT")

        def rms_stats(tag, rd=None):
            if rd is None:
                rd = rstd
            B_sq = [Buf(f"sq{tag}{i}") for i in range(2)]
            B_rstd = Buf(f"rstd{tag}")
            bk0, bk1 = aux_bank(), aux_bank()
            bks = [bk0, bk1]
            for c in range(KC):
                i = c % 2
                if c % 4 < 2:
                    emit(ACT, lambda e, c=c, i=i: e.activation(out=sqv[:, i, :], in_=xT[:, c, :], func=AF.Square),
                         reads=[B_x[c][0], B_x[c][1]], writes=[B_sq[i]])
                else:
                    emit(DVE, lambda e, c=c, i=i: e.tensor_tensor(out=sqv[:, i, :], in0=xT[:, c, :], in1=xT[:, c, :], op=ALU.mult),
                         reads=[B_x[c][0], B_x[c][1]], writes=[B_sq[i]])
                fns = []
                for b in range(2):
                    fns.append(lambda e, b=b, i=i, c=c: e.matmul(
                        psb[bks[b]][:, :], ones_bf[:], sqv[:, i, b * 512:(b + 1) * 512],
                        start=(c == 0), stop=(c == KC - 1)))
                emit_group(PE, fns, reads=[B_sq[i], B_cst], writes=[B_ps[bk0], B_ps[bk1]])
            for b in range(2):
                emit(ACT, lambda e, b=b: e.activation(out=rd[:, b * 512:(b + 1) * 512], in_=psb[bks[b]][:, :],
                                                     func=AF.Ln, scale=1.0 / D, bias=epst[:, 0:1]),
                     reads=[B_ps[bks[b]], B_cst], writes=[B_rstd])
                emit(ACT, lambda e, b=b: e.activation(out=rd[:, b * 512:(b + 1) * 512], in_=rd[:, b * 512:(b + 1) * 512],
                                                     func=AF.Exp, scale=-0.5),
                     reads=[B_rstd], writes=[B_rstd])
            return B_rstd

        def dump_f32(src3):
            for c in range(KC):
                emit(SP, lambda e, c=c: e.dma_start(out=outT_d[:, c, :], in_=src3[:, c, :]),
                     reads=[B_x[c][0], B_x[c][1]], dsem=out_sem)

        def finish():
            SP.ops.append(("w", out_sem.sem, 16 * out_sem.count))

            @block.tensor
            def _(h):
                replay(PE, h)

            @block.scalar
            def _(h):
                replay(ACT, h)

            @block.vector
            def _(h):
                replay(DVE, h)

            @block.gpsimd
            def _(h):
                replay(POOL, h)

            @block.sync
            def _(h):
                replay(SP, h)

        def dump_bf16(src3, bufs):
            barrier()
            for c in range(KC):
                emit(DVE, lambda e, c=c: e.tensor_copy(out=xT[:, c, :], in_=src3[:, c, :]),
                     reads=bufs, writes=[B_x[c][0], B_x[c][1]])
            dump_f32(xT)

        B_hs = Buf("hs")
        sqh = arb[:, 0:256].rearrange("p (c t) -> p c t", c=KC)
        rsh = small[:, 0:16]
        th = rt[:, 0:256].rearrange("p (c t) -> p c t", c=KC)
        B_rstd0 = rms_stats("n1_0")
        emit(ACT, lambda e: e.activation(out=sqh, in_=xh[:], func=AF.Square), reads=[B_par], writes=[B_hs])
        bk = aux_bank()
        fns = [lambda e, c=c, bk=bk: e.matmul(psb[bk][:, 0:HALO], ones_bf[:], sqh[:, c, :],
                                              start=(c == 0), stop=(c == KC - 1)) for c in range(KC)]
        emit_group(PE, fns, reads=[B_hs, B_cst], writes=[B_ps[bk]])
        emit(ACT, lambda e, bk=bk: e.activation(out=rsh, in_=psb[bk][:, 0:HALO], func=AF.Sqrt,
                                                scale=1.0 / D, bias=epst[:, 0:1]),
             reads=[B_ps[bk], B_cst], writes=[B_hs])
        emit(DVE, lambda e: e.reciprocal(out=rsh, in_=rsh), reads=[B_hs], writes=[B_hs])
        emit(DVE, lambda e: e.tensor_tensor(out=th, in0=xh[:], in1=rsh.unsqueeze(1).to_broadcast([128, KC, HALO]),
                                            op=ALU.mult), reads=[B_hs, B_par], writes=[B_hs])
        ada_slots(0, 0, 16)
        for l in range(2):
            emit(DVE, lambda e, l=l: e.scalar_tensor_tensor(out=a1[:, l, :], in0=mod(l, 1), scalar=1.0, in1=nmix[:, l, :],
                                                            op0=ALU.add, op1=ALU.mult),
                 reads=[B_mod[l], B_par], writes=[B_mod[l]])
            if l == 0:
                B_rstd = B_rstd0
                emit(DVE, lambda e: e.tensor_tensor(out=th, in0=th, in1=a1[:, 0, :].unsqueeze(2).to_broadcast([128, KC, HALO]),
                                                    op=ALU.mult), reads=[B_hs, B_mod[0]], writes=[B_hs])
                emit(DVE, lambda e: e.tensor_tensor(out=hh[:], in0=th, in1=mod(0, 0).unsqueeze(2).to_broadcast([128, KC, HALO]),
                                                    op=ALU.add), reads=[B_hs, B_mod[0]], writes=[B_hh])
            else:
                barrier()
                B_rstd = rms_stats(f"n1_{l}")
            B_tmp = [Buf(f"tmp{i}") for i in range(2)]
            for c in range(KC):
                for b in range(2):
                    i = b
                    emit(DVE, lambda e, c=c, b=b, i=i, l=l: e.scalar_tensor_tensor(
                        out=tmpv[:, i, :], in0=xT[:, c, b * 512:(b + 1) * 512], scalar=a1[:, l, c:c + 1],
                        in1=rstd[:, b * 512:(b + 1) * 512], op0=ALU.mult, op1=ALU.mult),
                        reads=[B_x[c][b], B_rstd, B_mod[l]], writes=[B_tmp[i]])
                    emit(ACT, lambda e, c=c, b=b, i=i, l=l: e.activation(
                        out=hT[:, c, b * 512:(b + 1) * 512], in_=tmpv[:, i, :], func=AF.Identity,
                        bias=mod(l, 0)[:, c:c + 1], scale=1.0),
                        reads=[B_tmp[i], B_mod[l]], writes=[B_h[c]])
            if stop == f"h1_{l}":
                dump_bf16(hT, B_h)
                return nc, finish()
            barrier()

            ymix = arb[:, 0:16384].rearrange("p (c t) -> p c t", c=KC)
            B_ym = [Buf(f"ym{c}") for c in range(KC)]
            if l == 0:
                zb = arf[:, 0:1040]
                sA = arf[:, 1040:2080]
                sB = arf[:, 2080:3120]
                t16 = arf[:, 3120:3136]
                B_zb, B_sA, B_sB, B_t16 = Buf("zb"), Buf("sA"), Buf("sB"), Buf("t16")
                pooled = arb[:, 16384:18432].rearrange("p (c t) -> p c t", c=2)
                B_pl = [Buf("pl0"), Buf("pl1")]
                flag = pc[:, 0:1]

                def zgroup(sv, q, with_halo):
                    bks = [main_bank(), main_bank()]
                    fns = []
                    for kc in range(KC):
                        for b in range(2):
                            fns.append((lambda e, kc=kc, b=b, bks=bks: e.matmul(
                                psb[bks[b]][:, :], sv[:, kc, q * 128:(q + 1) * 128], hT[:, kc, b * 512:(b + 1) * 512],
                                start=(kc == 0), stop=(kc == KC - 1)), [B_h[kc]]))
                    bh = None
                    if with_halo:
                        bh = aux_bank()
                        for kc in range(KC):
                            fns.append(lambda e, kc=kc, bh=bh: e.matmul(
                                psb[bh][:, 0:HALO], sv[:, kc, q * 128:(q + 1) * 128], hh[:, kc, :],
                                start=(kc == 0), stop=(kc == KC - 1)))
                    return bks, bh, fns

                mod_sched = list(range(16, 32))
                def pool_mm(g):
                    for oc in range(2):
                        bks = [main_bank(), main_bank()]
                        fns = []
                        for kc in range(2):
                            for b in range(2):
                                fns.append(lambda e, kc=kc, b=b, oc=oc, g=g, bks=bks: e.matmul(
                                    psb[bks[b]][:, :], wpool[:, g, kc, oc * 128:(oc + 1) * 128],
                                    pooled[:, kc, b * 512:(b + 1) * 512], start=(kc == 0), stop=(kc == 1)))
                        emit_group(PE, fns, reads=[B_parg] + B_pl, writes=[B_ps[bks[0]], B_ps[bks[1]]])
                        for b in range(2):
                            emit(ACT, lambda e, b=b, oc=oc, g=g, bks=bks: e.activation(
                                out=ymix[:, 2 * g + oc, b * 512:(b + 1) * 512], in_=psb[bks[b]][:, :], func=AF.Identity,
                                scale=pscale[:, 2 * g + oc:2 * g + oc + 1]),
                                reads=[B_ps[bks[b]], B_par], writes=[B_ym[2 * g + oc]])
                for g in range(4):
                    w = 2 ** (g + 1)
                    for half in range(2):
                        c = 2 * g + half
                        if half == 0:
                            bs, sv = next_slot(w_in_even_d[:, 256 * g:256 * (g + 1)].rearrange("(kc p) n -> p kc n", p=128),
                                               [128, KC, 256])
                        bks, bh, fns = zgroup(sv, half, True)
                        emit_group(PE, fns, reads=[bs, B_hh], writes=[B_ps[bks[0]], B_ps[bks[1]], B_ps[bh]])
                        if half == 0 and g >= 1:
                            pool_mm(g - 1)
                        emit(ACT, lambda e, bh=bh: e.activation(out=zb[:, 0:HALO], in_=psb[bh][:, 0:HALO], func=AF.Identity,
                                                                scale=flag),
                             reads=[B_ps[bh], B_par], writes=[B_zb])
                        for b in range(2):
                            emit(ACT, lambda e, b=b, bks=bks: e.activation(
                                out=zb[:, HALO + b * 512:HALO + (b + 1) * 512], in_=psb[bks[b]][:, :], func=AF.Copy),
                                reads=[B_ps[bks[b]]], writes=[B_zb])
                        emit(DVE, lambda e: e.tensor_tensor(out=sA[:, 1:1040], in0=zb[:, 1:1040], in1=zb[:, 0:1039], op=ALU.add),
                             reads=[B_zb], writes=[B_sA])
                        cur, curB = sA, B_sA
                        oth, othB = sB, B_sB
                        k = 2
                        while k < w:
                            emit(DVE, lambda e, k=k, cur=cur, oth=oth: e.tensor_tensor(
                                out=oth[:, 2 * k - 1:1040], in0=cur[:, 2 * k - 1:1040], in1=cur[:, k - 1:1040 - k], op=ALU.add),
                                reads=[curB], writes=[othB])
                            cur, curB, oth, othB = oth, othB, cur, curB
                            k *= 2
                        emit(DVE, lambda e, cur=cur, half=half, w=w: e.scalar_tensor_tensor(
                            out=pooled[:, half, :], in0=cur[:, HALO:1040], scalar=1.0 / w, in1=zb[:, HALO:1040],
                            op0=ALU.mult, op1=ALU.subtract),
                            reads=[curB, B_zb], writes=[B_pl[half]])
                        emit(DVE, lambda e, cur=cur, g=g: e.tensor_tensor(
                            out=t16, in0=cur[:, HALO:2 * HALO], in1=pc[:, 1 + 16 * g:17 + 16 * g], op=ALU.mult),
                            reads=[curB, B_par], writes=[B_t16])
                        emit(DVE, lambda e, half=half: e.tensor_tensor(
                            out=pooled[:, half, 0:HALO], in0=t16, in1=zb[:, HALO:2 * HALO], op=ALU.subtract),
                            reads=[B_t16, B_zb], writes=[B_pl[half]])
                        ada_slots(0, mod_sched.pop(0), 1)
                zv = [arf[:, 0:1040], arf[:, 1040:2080]]
                accs = [arf[:, 2080:3104], rt[:, 0:1024]]
                B_zv = [B_zb, B_sA]
                B_ac = [B_sB, Buf("ac1")]

                def conv_slot(base, pr):
                    return next_slot(w_in_even_d[:, base + 256 * pr:base + 256 * (pr + 1)].rearrange("(kc p) n -> p kc n", p=128),
                                     [128, KC, 256])

                def some_ada(n):
                    for _ in range(n):
                        if mod_sched:
                            ada_slots(0, mod_sched.pop(0), 1)

                for pr in range(4):
                    sl = conv_slot(1024, pr)
                    for q in range(2):
                        bks, bh, fns = zgroup(sl[1], q, True)
                        emit_group(PE, fns, reads=[sl[0], B_hh], writes=[B_ps[bks[0]], B_ps[bks[1]], B_ps[bh]])
                        if pr == 0 and q == 0:
                            pool_mm(3)
                        emit(ACT, lambda e, bh=bh, q=q: e.activation(out=zv[q][:, 0:HALO], in_=psb[bh][:, 0:HALO], func=AF.Identity,
                                                                     scale=flag),
                             reads=[B_ps[bh], B_par], writes=[B_zv[q]])
                        for b in range(2):
                            emit(ACT, lambda e, b=b, bks=bks, q=q: e.activation(
                                out=zv[q][:, HALO + b * 512:HALO + (b + 1) * 512], in_=psb[bks[b]][:, :], func=AF.Copy),
                                reads=[B_ps[bks[b]]], writes=[B_zv[q]])
                    some_ada(0)
                    sl = conv_slot(3072, pr)
                    for q in range(2):
                        hd = 2 * pr + q
                        bks, bh, fns = zgroup(sl[1], q, True)
                        emit_group(PE, fns, reads=[sl[0], B_hh], writes=[B_ps[bks[0]], B_ps[bks[1]], B_ps[bh]])
                        emit(DVE, lambda e, bh=bh, q=q: e.tensor_tensor(out=zv[q][:, 0:HALO], in0=psb[bh][:, 0:HALO],
                                                                        in1=zv[q][:, 0:HALO], op=ALU.mult),
                             reads=[B_ps[bh], B_zv[q]], writes=[B_zv[q]])
                        for b in range(2):
                            emit(DVE, lambda e, b=b, bks=bks, q=q: e.tensor_tensor(
                                out=zv[q][:, HALO + b * 512:HALO + (b + 1) * 512], in0=psb[bks[b]][:, :],
                                in1=zv[q][:, HALO + b * 512:HALO + (b + 1) * 512], op=ALU.mult),
                                reads=[B_ps[bks[b]], B_zv[q]], writes=[B_zv[q]])
                        emit(ACT, lambda e, hd=hd, q=q: e.activation(out=accs[q], in_=zv[q][:, HALO:HALO + NT], func=AF.Identity,
                                                                     scale=convw[:, 2, hd:hd + 1]),
                             reads=[B_zv[q], B_par], writes=[B_ac[q]])
                        for k in (1, 0):
                            sh = 2 - k
                            emit(DVE, lambda e, hd=hd, k=k, sh=sh, q=q: e.scalar_tensor_tensor(
                                out=accs[q], in0=zv[q][:, HALO - sh:HALO - sh + NT], scalar=convw[:, k, hd:hd + 1],
                                in1=accs[q], op0=ALU.mult, op1=ALU.add),
                                reads=[B_zv[q], B_ac[q], B_par], writes=[B_ac[q]])
                    some_ada(0)
                    sl = conv_slot(2048, pr)
                    for q in range(2):
                        hd = 2 * pr + q
                        bks, bh, fns = zgroup(sl[1], q, False)
                        emit_group(PE, fns, reads=[sl[0]], writes=[B_ps[bks[0]], B_ps[bks[1]]])
                        for b in range(2):
                            emit(DVE, lambda e, b=b, bks=bks, hd=hd, q=q: e.tensor_tensor(
                                out=ymix[:, 8 + hd, b * 512:(b + 1) * 512], in0=psb[bks[b]][:, :],
                                in1=accs[q][:, b * 512:(b + 1) * 512], op=ALU.mult),
                                reads=[B_ps[bks[b]], B_ac[q]], writes=[B_ym[8 + hd]])
                    some_ada(2)
                while mod_sched:
                    ada_slots(0, mod_sched.pop(0), 1)
                w_out_d = w_out_even_d
            else:
                vq = arb[:, 0:16384].rearrange("p (c i f) -> p c i f", c=KC, i=8)
                ug = arb[:, 16384:17408].rearrange("p (b t) -> p b t", b=2)
                B_ug = [Buf("ug0"), Buf("ug1")]
                sgn = arf[:, 0:2048]
                B_sgn = Buf("sgn")
                for t_ in (PE.last, ACT.last, DVE.last):
                    SP.wait(t_)
                emit(SP, lambda e: e.dma_start(out=sgn, in_=sgn_d[0, :].partition_broadcast(128)), writes=[B_sgn], dsem=misc_sem)
                B_vq = [Buf(f"vq{c}") for c in range(KC)]
                sqj = arb[:, 17408:17920].rearrange("p (u q f) -> p u q f", u=2, q=2)
                B_sqj = [Buf("sqj0"), Buf("sqj1")]
                sspart = rt[:, 0:64].rearrange("p (i j) -> p i j", i=8)
                B_ssp = Buf("ssp")
                for j in range(8):
                    bs, sv = next_slot(w_in_odd_d[:, D + 256 * j:D + 256 * (j + 1)].rearrange("(kc p) n -> p kc n", p=128),
                                       [128, KC, 256])
                    for i in range(8):
                        bk = main_bank()
                        fns = [(lambda e, kc=kc, i=i, bk=bk, sv=sv: e.matmul(
                            psb[bk][:, 0:256], hT[:, kc, i * 128:(i + 1) * 128], sv[:, kc, :],
                            start=(kc == 0), stop=(kc == KC - 1)), [B_h[kc]]) for kc in range(KC)]
                        emit_group(PE, fns, reads=[bs], writes=[B_ps[bk]])
                        emit(ACT, lambda e, j=j, i=i, bk=bk: e.activation(
                            out=vq[:, 2 * j:2 * j + 2, i, :], in_=psb[bk][:, 0:256].rearrange("p (q f) -> p q f", q=2),
                            func=AF.Gelu_apprx_tanh),
                            reads=[B_ps[bk]], writes=[B_vq[2 * j], B_vq[2 * j + 1]])
                        emit(DVE, lambda e, j=j, i=i: e.scalar_tensor_tensor(
                            out=sqj[:, (i % 2), :, :], in0=vq[:, 2 * j:2 * j + 2, i, :], scalar=1.0, in1=vq[:, 2 * j:2 * j + 2, i, :],
                            op0=ALU.mult, op1=ALU.mult, accum_out=sspart[:, i, j:j + 1]),
                            reads=[B_vq[2 * j], B_vq[2 * j + 1]], writes=[B_sqj[i % 2], B_ssp])
                if stop == "vq0":
                    dump_bf16(arb[:, 0:16384].rearrange("p (c t) -> p c t", c=KC), B_vq)
                    return nc, finish()
                ssv = small[:, 16:24]
                rsv = small[:, 24:32]
                B_ssv = Buf("ssv")
                emit(DVE, lambda e: e.tensor_reduce(out=ssv, in_=sspart, axis=AX.X, op=ALU.add), reads=[B_ssp], writes=[B_ssv])
                emit(ACT, lambda e: e.activation(out=rsv, in_=ssv, func=AF.Sqrt, scale=1.0 / D, bias=epst[:, 0:1]),
                     reads=[B_ssv, B_cst], writes=[B_ssv])
                emit(DVE, lambda e: e.reciprocal(out=rsv, in_=rsv), reads=[B_ssv], writes=[B_ssv])

                def normalize_chunk(c):
                    emit(DVE, lambda e, c=c: e.tensor_tensor(out=vq[:, c, :, :], in0=vq[:, c, :, :],
                                                             in1=rsv.unsqueeze(2).to_broadcast([128, 8, 128]), op=ALU.mult),
                         reads=[B_vq[c], B_ssv], writes=[B_vq[c]])
                    emit(DVE, lambda e, c=c: e.tensor_tensor(out=vq[:, c, :, :], in0=vq[:, c, :, :],
                                                             in1=sgn[:, c * 128:(c + 1) * 128].unsqueeze(1).to_broadcast([128, 8, 128]),
                                                             op=ALU.mult),
                         reads=[B_vq[c], B_sgn], writes=[B_vq[c]])

                if stop == "vq1":
                    for c in range(KC):
                        normalize_chunk(c)
                    dump_bf16(arb[:, 0:16384].rearrange("p (c t) -> p c t", c=KC), B_vq)
                    return nc, finish()
                for j in range(8):
                    bs, sv = next_slot(w_in_odd_d[:, 256 * j:256 * (j + 1)].rearrange("(kc p) n -> p kc n", p=128),
                                       [128, KC, 256])
                    for q in range(2):
                        c = 2 * j + q
                        hd = c // 2
                        normalize_chunk(c)
                        bks = [main_bank(), main_bank()]
                        fns = []
                        for kc in range(KC):
                            for b in range(2):
                                fns.append(lambda e, kc=kc, b=b, q=q, sv=sv, bks=bks: e.matmul(
                                    psb[bks[b]][:, :], sv[:, kc, q * 128:(q + 1) * 128], hT[:, kc, b * 512:(b + 1) * 512],
                                    start=(kc == 0), stop=(kc == KC - 1)))
                        emit_group(PE, fns, reads=[bs] + B_h, writes=[B_ps[bks[0]], B_ps[bks[1]]])
                        for b in range(2):
                            emit(ACT, lambda e, b=b, bks=bks: e.activation(
                                out=ug[:, b, :], in_=psb[bks[b]][:, :], func=AF.Gelu_apprx_tanh),
                                reads=[B_ps[bks[b]]], writes=[B_ug[b]])
                        sbk = [main_bank(), main_bank()]
                        fns = []
                        for i in range(8):
                            o = psb[sbk[i // 4]][:, (i % 4) * 128:(i % 4 + 1) * 128]
                            fns.append(lambda e, o=o, hd=hd: e.matmul(o, ones_bf[:], bmat[:, hd, :], start=True, stop=False))
                            fns.append(lambda e, o=o, hd=hd, c=c, i=i: e.matmul(o, vq[:, c, i, :], wspT[:, hd, :],
                                                                                start=False, stop=True))
                        emit_group(PE, fns, reads=[B_vq[c], B_wsp, B_parg, B_cst], writes=[B_ps[sbk[0]], B_ps[sbk[1]]])
                        for b in range(2):
                            emit(DVE, lambda e, b=b, c=c, sbk=sbk: e.tensor_tensor(
                                out=ymix[:, c, b * 512:(b + 1) * 512], in0=psb[sbk[b]][:, :], in1=ug[:, b, :], op=ALU.mult),
                                reads=[B_ps[sbk[b]], B_ug[b]], writes=[B_vq[c]])
                B_ym = B_vq
                w_out_d = w_out_odd_d
            if stop == f"ymix{l}":
                dump_bf16(ymix, B_ym)
                return nc, finish()

            mix_end = [PE.last, ACT.last, DVE.last]
            for j in range(8):
                bs, sv = next_slot(w_out_d[:, 256 * j:256 * (j + 1)].rearrange("(kc p) n -> p kc n", p=128), [128, KC, 256])
                for q in range(2):
                    dc = 2 * j + q
                    bks = [main_bank(), main_bank()]
                    fns = []
                    for kc in range(KC):
                        for b in range(2):
                            fns.append(lambda e, kc=kc, b=b, q=q, sv=sv, bks=bks: e.matmul(
                                psb[bks[b]][:, :], sv[:, kc, q * 128:(q + 1) * 128], ymix[:, kc, b * 512:(b + 1) * 512],
                                start=(kc == 0), stop=(kc == KC - 1)))
                    emit_group(PE, fns, reads=[bs] + B_ym, writes=[B_ps[bks[0]], B_ps[bks[1]]])
                    for b in range(2):
                        emit(DVE, lambda e, b=b, dc=dc, bks=bks, l=l: e.scalar_tensor_tensor(
                            out=xT[:, dc, b * 512:(b + 1) * 512], in0=psb[bks[b]][:, :], scalar=mod(l, 2)[:, dc:dc + 1],
                            in1=xT[:, dc, b * 512:(b + 1) * 512], op0=ALU.mult, op1=ALU.add),
                            reads=[B_ps[bks[b]], B_mod[l], B_x[dc][b]], writes=[B_x[dc][b]])
                if l == 0:
                    ada_slots(0, 32 + j, 1)
            if stop == f"mix{l}":
                dump_f32(xT)
                return nc, finish()

            emit(DVE, lambda e, l=l: e.scalar_tensor_tensor(out=a2[:, l, :], in0=mod(l, 4), scalar=1.0, in1=nffn[:, l, :],
                                                            op0=ALU.add, op1=ALU.mult),
                 reads=[B_mod[l], B_par], writes=[B_mod[l]])
            for e_ in (ACT, DVE):
                for t_ in mix_end:
                    e_.wait(t_)
            B_rstd = rms_stats(f"n2_{l}")
            B_tmp = [Buf(f"tmpb{i}") for i in range(2)]
            B_hf = [Buf(f"hf{i}") for i in range(2)]
            lbk = [aux_bank(), aux_bank()]
            for c in range(KC):
                for b in range(2):
                    i = b
                    emit(DVE, lambda e, c=c, b=b, i=i, l=l: e.scalar_tensor_tensor(
                        out=tmpv[:, i, :], in0=xT[:, c, b * 512:(b + 1) * 512], scalar=a2[:, l, c:c + 1],
                        in1=rstd[:, b * 512:(b + 1) * 512], op0=ALU.mult, op1=ALU.mult),
                        reads=[B_x[c][b], B_rstd, B_mod[l]], writes=[B_tmp[i]])
                    emit(ACT, lambda e, c=c, b=b, i=i, l=l: e.activation(
                        out=hfv[:, i, :], in_=tmpv[:, i, :], func=AF.Identity, bias=mod(l, 3)[:, c:c + 1], scale=1.0),
                        reads=[B_tmp[i], B_mod[l]], writes=[B_hf[i]])
                    emit(DVE, lambda e, c=c, b=b, i=i: e.tensor_copy(out=hT[:, c, b * 512:(b + 1) * 512], in_=hfv[:, i, :]),
                         reads=[B_hf[i]], writes=[B_h[c]])
                    emit_group(PE, [lambda e, c=c, b=b, i=i: e.matmul(
                        psb[lbk[b]][0:16, :], wr[:, c, :], hfv[:, i, :], start=(c == 0), stop=(c == KC - 1))],
                        reads=[B_hf[i], B_par], writes=[B_ps[lbk[b]]])
            if stop == f"h2_{l}":
                dump_bf16(hT, B_h)
                return nc, finish()
            B_lg = Buf("lg")
            for b in range(2):
                emit(ACT, lambda e, b=b: e.activation(out=lgT[:, b * 512:(b + 1) * 512], in_=psb[lbk[b]][0:16, :], func=AF.Sigmoid),
                     reads=[B_ps[lbk[b]]], writes=[B_lg])
            bk = aux_bank()
            fns = [lambda e, i=i, bk=bk: e.transpose(psb[bk][:, i * 16:(i + 1) * 16], lgT[:, i * 128:(i + 1) * 128], ident[0:16, 0:16])
                   for i in range(8)]
            emit_group(PE, fns, reads=[B_lg, B_par], writes=[B_ps[bk]])
            aff = rt[:, 0:128]
            sel = rt[:, 128:256]
            eq1 = rt[:, 256:384]
            sel2 = rt[:, 384:512]
            ge2 = rt[:, 512:640]
            wts = rt[:, 640:768]
            comb = rt[:, 768:896]
            m1 = rt[:, 896:928]
            m2 = rt[:, 928:960]
            gs = rt[:, 960:992]
            gmax = rt[:, 992:1000]
            gmask = rt[:, 1000:1032]
            den = rt[:, 1032:1040]
            B_rt = Buf("rt")

            def g4(ap):
                return ap.rearrange("p (a k) -> p a k", k=4)

            def bl(ap, n):
                return ap.unsqueeze(2).to_broadcast([128, ap.shape[1], n])

            R = dict(reads=[B_rt], writes=[B_rt])
            emit(DVE, lambda e, bk=bk: e.tensor_copy(out=aff, in_=psb[bk][:, 0:128]), reads=[B_ps[bk]], writes=[B_rt])
            emit(DVE, lambda e: e.tensor_tensor(out=sel.rearrange("p (i x) -> p i x", i=8), in0=aff.rearrange("p (i x) -> p i x", i=8),
                                                in1=rbias[:].unsqueeze(1).to_broadcast([128, 8, NE]), op=ALU.add),
                 reads=[B_rt, B_par], writes=[B_rt])
            emit(DVE, lambda e: e.tensor_reduce(out=m1, in_=g4(sel), axis=AX.X, op=ALU.max), **R)
            emit(DVE, lambda e: e.tensor_tensor(out=g4(eq1), in0=g4(sel), in1=bl(m1, 4), op=ALU.is_equal), **R)
            emit(DVE, lambda e: e.scalar_tensor_tensor(out=sel2, in0=eq1, scalar=-1e30, in1=sel, op0=ALU.mult, op1=ALU.add), **R)
            emit(DVE, lambda e: e.tensor_reduce(out=m2, in_=g4(sel2), axis=AX.X, op=ALU.max), **R)
            emit(DVE, lambda e: e.tensor_tensor(out=gs, in0=m1, in1=m2, op=ALU.add), **R)
            emit(DVE, lambda e: e.tensor_reduce(out=gmax, in_=g4(gs), axis=AX.X, op=ALU.max), **R)
            emit(DVE, lambda e: e.tensor_tensor(out=g4(gmask), in0=g4(gs), in1=bl(gmax, 4), op=ALU.is_equal), **R)
            emit(DVE, lambda e: e.tensor_tensor(out=g4(ge2), in0=g4(sel), in1=bl(m2, 4), op=ALU.is_ge), **R)
            emit(DVE, lambda e: e.tensor_tensor(out=g4(ge2), in0=g4(ge2), in1=bl(gmask, 4), op=ALU.mult), **R)
            emit(DVE, lambda e: e.tensor_tensor(out=wts, in0=aff, in1=ge2, op=ALU.mult), **R)
            emit(DVE, lambda e: e.tensor_reduce(out=den, in_=wts.rearrange("p (i x) -> p i x", i=8), axis=AX.X, op=ALU.add), **R)
            emit(DVE, lambda e: e.reciprocal(out=den, in_=den), **R)
            emit(DVE, lambda e: e.tensor_tensor(out=comb.rearrange("p (i x) -> p i x", i=8), in0=wts.rearrange("p (i x) -> p i x", i=8),
                                                in1=bl(den, NE), op=ALU.mult), **R)
            hib = arb[:, 0:128].rearrange("p (i x) -> p i x", i=8)
            chl = rt[:, 256:512].rearrange("p (i x) -> p i x", i=8)
            comb3 = comb.rearrange("p (i x) -> p i x", i=8)
            emit(DVE, lambda e: e.tensor_copy(out=hib, in_=comb3), **R)
            emit(DVE, lambda e: e.tensor_copy(out=chl[:, :, 0:16], in_=hib), **R)
            emit(DVE, lambda e: e.tensor_tensor(out=chl[:, :, 16:32], in0=comb3, in1=chl[:, :, 0:16], op=ALU.subtract), **R)
            bkc = [aux_bank(), aux_bank()]
            fns = [lambda e, i=i: e.transpose(psb[bkc[i // 4]][0:32, (i % 4) * 128:(i % 4 + 1) * 128], chl[:, i, :], ident[:])
                   for i in range(8)]
            emit_group(PE, fns, reads=[B_rt, B_par], writes=[B_ps[bkc[0]], B_ps[bkc[1]]])
            for b in range(2):
                emit(ACT, lambda e, b=b: e.activation(out=combT[:, b * 512:(b + 1) * 512], in_=psb[bkc[b]][0:32, :], func=AF.Copy),
                     reads=[B_ps[bkc[b]]], writes=[B_combT])
            if stop == f"comb{l}":
                barrier()
                emit(DVE, lambda e: e.memset(xT[:, 0, :], 0.0), writes=[B_x[0][0], B_x[0][1]])
                emit(DVE, lambda e: e.tensor_copy(out=xT[0:32, 0, :], in_=combT[:]), reads=[B_combT], writes=[B_x[0][0], B_x[0][1]])
                dump_f32(xT)
                return nc, finish()
            barrier()

            aT = arb[:, 0:8192].rearrange("p (u f t) -> p u f t", u=2, f=4)
            sg = arf[:, 2048:3072].rearrange("p (u t) -> p u t", u=2)
            cb = arf[:, 0:2048].rearrange("p (u t) -> p u t", u=2)
            B_aT = [[Buf(f"aT{u}_{f}") for f in range(4)] for u in range(2)]
            B_sg = [Buf("sg0"), Buf("sg1")]
            B_cb = [Buf("cb0"), Buf("cb1")]
            B_sel = [Buf("sel0"), Buf("sel1")]
            nxt_ada = ([(0, j) for j in range(40, 48)] + [(1, j) for j in range(48)]) if l == 0 else []
            set_ring(RING + 2, gate_toks=[PE.last, ACT.last, DVE.last])
            sgi = [0]

            def gate_up(ex):
                u = ex % 2
                def emit_cb():
                    bks = [aux_bank(), aux_bank()]
                    emit(DVE, lambda e, ex=ex, u=u: e.tensor_tensor(out=selmat[:, u, :], in0=ident[0:32, ex:ex + 1].to_broadcast([32, 128]),
                                                                    in1=ident[0:32, 16 + ex:17 + ex].to_broadcast([32, 128]), op=ALU.add),
                         reads=[B_par], writes=[B_sel[u]])
                    fns = [lambda e, b=b, u=u, bks=bks: e.matmul(psb[bks[b]][:, :], selmat[:, u, :], combT[:, b * 512:(b + 1) * 512],
                                                                 start=True, stop=True) for b in range(2)]
                    emit_group(PE, fns, reads=[B_combT, B_sel[u]], writes=[B_ps[bks[0]], B_ps[bks[1]]])
                    for b in range(2):
                        emit(ACT, lambda e, b=b, u=u, bks=bks: e.activation(out=cb[:, u, b * 512:(b + 1) * 512], in_=psb[bks[b]][:, :],
                                                                            func=AF.Copy),
                             reads=[B_ps[bks[b]]], writes=[B_cb[u]])
                for half in range(2):
                    gs_ = next_slot(wg_d[l, ex][:, 256 * half:256 * (half + 1)].rearrange("(kc p) n -> p kc n", p=128), [128, KC, 256])
                    us_ = next_slot(wu_d[l, ex][:, 256 * half:256 * (half + 1)].rearrange("(kc p) n -> p kc n", p=128), [128, KC, 256])
                    for q in range(2):
                        fc = 2 * half + q
                        for b in range(2):
                            bg, bu = main_bank(), main_bank()
                            fns = []
                            for kc in range(KC):
                                fns.append((lambda e, kc=kc, q=q, b=b, bg=bg, sv=gs_[1]: e.matmul(
                                    psb[bg][:, :], sv[:, kc, q * 128:(q + 1) * 128], hT[:, kc, b * 512:(b + 1) * 512],
                                    start=(kc == 0), stop=(kc == KC - 1)), [B_h[kc]]))
                            for kc in range(KC):
                                fns.append(lambda e, kc=kc, q=q, b=b, bu=bu, sv=us_[1]: e.matmul(
                                    psb[bu][:, :], sv[:, kc, q * 128:(q + 1) * 128], hT[:, kc, b * 512:(b + 1) * 512],
                                    start=(kc == 0), stop=(kc == KC - 1)))
                            emit_group(PE, fns, reads=[gs_[0], us_[0]], writes=[B_ps[bg], B_ps[bu]])
                            if half == 0 and q == 0 and b == 0:
                                emit_cb()
                            si = sgi[0]
                            sgi[0] = 1 - si
                            emit(ACT, lambda e, bg=bg, si=si: e.activation(out=sg[:, si, :], in_=psb[bg][:, :], func=AF.Silu),
                                 reads=[B_ps[bg]], writes=[B_sg[si]])
                            emit(DVE, lambda e, si=si, u=u, b=b: e.tensor_tensor(
                                out=sg[:, si, :], in0=sg[:, si, :], in1=cb[:, u, b * 512:(b + 1) * 512], op=ALU.mult),
                                reads=[B_sg[si], B_cb[u]], writes=[B_sg[si]])
                            emit(DVE, lambda e, si=si, u=u, b=b, fc=fc, bu=bu: e.tensor_tensor(
                                out=aT[:, u, fc, b * 512:(b + 1) * 512], in0=psb[bu][:, :], in1=sg[:, si, :], op=ALU.mult),
                                reads=[B_ps[bu], B_sg[si]], writes=[B_aT[u][fc]])

            def down(ex):
                u = ex % 2
                for half in range(2):
                    ds_ = next_slot(wd_d[l, ex][:, 1024 * half:1024 * (half + 1)].rearrange("(kc p) n -> p kc n", p=128), [128, 4, 1024])
                    for q in range(8):
                        dc = 8 * half + q
                        for b in range(2):
                            bk = main_bank()
                            fns = [lambda e, kc=kc, q=q, b=b, bk=bk, sv=ds_[1]: e.matmul(
                                psb[bk][:, :], sv[:, kc, q * 128:(q + 1) * 128], aT[:, u, kc, b * 512:(b + 1) * 512],
                                start=(kc == 0), stop=(kc == 3)) for kc in range(4)]
                            emit_group(PE, fns, reads=[ds_[0]] + B_aT[u], writes=[B_ps[bk]])
                            emit(DVE, lambda e, b=b, dc=dc, bk=bk, l=l: e.scalar_tensor_tensor(
                                out=xT[:, dc, b * 512:(b + 1) * 512], in0=psb[bk][:, :], scalar=mod(l, 5)[:, dc:dc + 1],
                                in1=xT[:, dc, b * 512:(b + 1) * 512], op0=ALU.mult, op1=ALU.add),
                                reads=[B_ps[bk], B_mod[l], B_x[dc][b]], writes=[B_x[dc][b]])

            nex = NE
            for ex in range(nex):
                gate_up(ex)
                if l == 0 and ex <= 1:
                    for _ in range(4):
                        l_, j_ = nxt_ada.pop(0)
                        ada_slots(l_, j_, 1)
                if ex >= 1:
                    down(ex - 1)
                if ex >= 2:
                    for _ in range(4):
                        if nxt_ada:
                            l_, j_ = nxt_ada.pop(0)
                            ada_slots(l_, j_, 1)
            down(nex - 1)
            while nxt_ada:
                l_, j_ = nxt_ada.pop(0)
                ada_slots(l_, j_, 1)
            set_ring(RING)
            if stop == f"moe{l}":
                dump_f32(xT)
                return nc, finish()

        rstd_fin = rt[:, 0:1024]
        B_rstd = rms_stats("fin", rd=rstd_fin)
        for c in range(KC):
            for b in range(2):
                emit(DVE, lambda e, c=c, b=b: e.scalar_tensor_tensor(
                    out=xT[:, c, b * 512:(b + 1) * 512], in0=xT[:, c, b * 512:(b + 1) * 512], scalar=fnorm[:, c:c + 1],
                    in1=rstd_fin[:, b * 512:(b + 1) * 512], op0=ALU.mult, op1=ALU.mult),
                    reads=[B_x[c][b], B_rstd, B_par], writes=[B_x[c][b]])
            emit(SP, lambda e, c=c: e.dma_start(out=outT_d[:, c, :], in_=xT[:, c, :]),
                 reads=[B_x[c][0], B_x[c][1]], dsem=out_sem)
        return nc, finish()


def _fm(v):
    v = np.asarray(v, np.float32)
    lead = v.shape[:-1]
    n = v.shape[-1] // 128
    return np.ascontiguousarray(np.moveaxis(v.reshape(*lead, n, 128), -1, 0))


def make_in_maps(inp):
    f = lambda a: np.ascontiguousarray(np.asarray(a, np.float32))
    x = f(inp["x"])
    shared = {
        "ada_w": f(inp["ada_w"]),
        "ada_bT": _fm(inp["ada_b"]),
        "nmixT": _fm(inp["norm_mix"]),
        "nffnT": _fm(inp["norm_ffn"]),
        "fnormT": _fm(inp["final_norm"]),
        "w_in_even": f(inp["w_in_even"][0]),
        "w_pool": f(inp["w_pool"][0]),
        "pscaleT": _fm(inp["pool_scale"][0]),
        "convwT": np.ascontiguousarray(np.transpose(f(inp["conv_w"][0]), (2, 0, 1))),
        "w_out_even": f(inp["w_out_even"][0]),
        "w_in_odd": f(inp["w_in_odd"][0]),
        "sgu_norm": f(inp["sgu_norm"]).reshape(1, D),
        "w_spatial": f(inp["w_spatial"][0]),
        "b_spatial": f(inp["b_spatial"][0]).reshape(1, 8 * 128),
        "w_out_odd": f(inp["w_out_odd"][0]),
        "w_router": f(inp["w_router"]),
        "router_bias": f(inp["router_bias"]).reshape(1, NE),
        "w_gate": f(inp["w_gate"]),
        "w_up": f(inp["w_up"]),
        "w_down": f(inp["w_down"]),
        "ident": np.eye(128, dtype=np.float32),
        "trilT": np.triu(np.ones((128, 128), np.float32)),
    }
    maps = []
    for core in range(NCORES):
        b, half = core // 2, core % 2
        t0 = half * NT
        xs = x[b, t0:t0 + NT]
        xT = np.ascontiguousarray(np.transpose(xs.reshape(NT, KC, 128), (2, 1, 0)))
        if half == 1:
            xh = x[b, t0 - HALO:t0]
        else:
            xh = np.zeros((HALO, D), np.float32)
        xhT = np.ascontiguousarray(np.transpose(xh.reshape(HALO, KC, 128), (2, 1, 0)))
        pcv = np.zeros((128, 65), np.float32)
        pcv[:, 0] = float(half)
        for g, w in enumerate((2, 4, 8, 16)):
            pos = np.arange(t0 + 1, t0 + 17, dtype=np.float32)
            pcv[:, 1 + 16 * g:17 + 16 * g] = (1.0 / np.minimum(pos, float(w)))[None, :]
        m = dict(shared)
        m["xT"] = xT
        m["xhT"] = xhT
        m["cT"] = _fm(inp["c"][b])
        m["pc"] = pcv
        maps.append(m)
    return maps


_NC_CACHE = {}


def kernel(**inputs):
    if "nc" not in _NC_CACHE:
        _NC_CACHE["nc"] = build()[0]
    nc = _NC_CACHE["nc"]
    maps = make_in_maps(inputs)
    res = run_bass_kernel_spmd(nc, maps, core_ids=list(range(NCORES)))
    out = np.empty((4, 2 * NT, D), np.float32)
    for core in range(NCORES):
        b, half = core // 2, core % 2
        oT = np.asarray(res.results[core]["outT"])
        out[b, half * NT:(half + 1) * NT] = np.transpose(oT, (2, 1, 0)).reshape(NT, D)
    return out
```
